# Optimizing a Trainium2 kernel written in Bass

```python
import math
import jax
import jax.numpy as jnp
from jax import lax
import numpy as np

D_MODEL = 1024
BATCH = 2
SEQ = 16384
DEPTH = 4

GRID_W = 64
CTX_LEN = 256
SSD_HEAD_DIM = 64
SSD_INNER = D_MODEL
SSD_HEADS = SSD_INNER // SSD_HEAD_DIM
SSD_GROUPS = 4
SSD_STATE = 128
SSD_CHUNK = 128
D_CONV = 5
SSD_NORM_EPS = 1e-5
XBC_W = SSD_INNER + 2 * SSD_GROUPS * SSD_STATE
POOL_W = D_MODEL // 2
POOL_WINDOWS = (2, 4, 8, 16)
POOL_GROUPS = len(POOL_WINDOWS)
POOL_GW = POOL_W // POOL_GROUPS
GMLP_W = D_MODEL // 2
GMLP_GROUPS = 4
GMLP_GW = GMLP_W // GMLP_GROUPS
GMLP_CHUNK = 128
LN_EPS = 1e-5
N_BRANCH = 3
MOE_GROUPS = 4
EXPERTS_PER_GROUP = 4
N_EXPERTS = MOE_GROUPS * EXPERTS_PER_GROUP
MOE_TOP_K = 2
D_EXPERT = D_MODEL // 2
RMS_EPS = 1e-6
OFF_Z = 0
OFF_XBC = OFF_Z + SSD_INNER
OFF_DT = OFF_XBC + XBC_W
OFF_POOL = OFF_DT + 2 * SSD_HEADS
OFF_GMLP = OFF_POOL + POOL_W
OFF_GATE = OFF_GMLP + 2 * GMLP_W
PROJ_W = OFF_GATE + N_BRANCH * D_MODEL

kernel_name = 'hybrid_ssd_pool_gmlp_hmoe_dit'

F32 = jnp.float32


def rmsnorm(x, g, eps=RMS_EPS):
    xf = x.astype(F32)
    y = xf * lax.rsqrt(jnp.mean(xf * xf, axis=-1, keepdims=True) + eps)
    return y * g.astype(F32)


def modulated_norm(x, g, shift, scale):
    return (rmsnorm(x, g) * (1.0 + scale.astype(F32)) + shift.astype(F32)).astype(x.dtype)


def dwconv(x, w, b):
    ch = x.shape[-1]
    y = lax.conv_general_dilated(
        x, w[:, None, :].astype(x.dtype), window_strides=(1,),
        padding=[(D_CONV // 2, D_CONV // 2)],
        dimension_numbers=('NWC', 'WIO', 'NWC'), feature_group_count=ch)
    return y + b.astype(y.dtype)


def ssd_inputs(cols, conv_w, conv_b, dt_bias):
    b, l, _ = cols.shape
    xbc = jax.nn.silu(dwconv(cols[..., :XBC_W], conv_w, conv_b))
    bc_w = SSD_GROUPS * SSD_STATE
    xs = xbc[..., :SSD_INNER].reshape(b, l, SSD_HEADS, SSD_HEAD_DIM)
    bm = xbc[..., SSD_INNER:SSD_INNER + bc_w].reshape(b, l, SSD_GROUPS, SSD_STATE)
    cm = xbc[..., SSD_INNER + bc_w:].reshape(b, l, SSD_GROUPS, SSD_STATE)
    dt = jax.nn.softplus(cols[..., XBC_W:].astype(F32).reshape(b, l, 2, SSD_HEADS)
                         + dt_bias.astype(F32))
    return xs, bm, cm, dt


def ssd_scan(x, dt, a, bm, cm, h0):
    b, l, nh, p = x.shape
    g, n = bm.shape[-2:]
    k = nh // g
    nc = l // SSD_CHUNK
    L = SSD_CHUNK
    xc = x.astype(F32).reshape(b, nc, L, g, k, p)
    dtc = dt.astype(F32).reshape(b, nc, L, g, k)
    bc = bm.astype(F32).reshape(b, nc, L, g, n)
    cc = cm.astype(F32).reshape(b, nc, L, g, n)
    acum = jnp.cumsum(dtc * a.astype(F32).reshape(g, k), axis=2)
    xdt = xc * dtc[..., None]
    seg = acum[:, :, :, None] - acum[:, :, None, :]
    tri = jnp.tril(jnp.ones((L, L), dtype=bool))[:, :, None, None]
    decay = jnp.exp(jnp.where(tri, seg, -jnp.inf))
    cb = jnp.einsum('bcign,bcjgn->bcijg', cc, bc)
    y_diag = jnp.einsum('bcijgk,bcjgkp->bcigkp', cb[..., None] * decay, xdt)
    decay_end = jnp.exp(acum[:, :, -1:] - acum)
    states = jnp.einsum('bcjgn,bcjgkp->bcgkpn', bc, xdt * decay_end[..., None])
    chunk_decay = jnp.exp(acum[:, :, -1])

    def step(h, inp):
        s, d = inp
        return h * d[..., None, None] + s, h

    h_last, h_in = lax.scan(step, h0.astype(F32).reshape(b, g, k, p, n),
                            (jnp.moveaxis(states, 1, 0), jnp.moveaxis(chunk_decay, 1, 0)))
    h_in = jnp.moveaxis(h_in, 0, 1)
    y_off = jnp.einsum('bcign,bcgkpn->bcigkp', cc, h_in) * jnp.exp(acum)[..., None]
    y = (y_diag + y_off).reshape(b, l, nh, p)
    return y, h_last.reshape(b, nh, p, n)


def ssd_bidir(xs, bm, cm, dt, a, h0_f, h0_b):
    flip = lambda t: jnp.flip(t, axis=1)
    y_f, h_f = ssd_scan(xs, dt[:, :, 0], a[0], bm, cm, h0_f)
    y_b, h_b = ssd_scan(flip(xs), flip(dt[:, :, 1]), a[1], flip(bm), flip(cm), h0_b)
    return y_f + flip(y_b), h_f, h_b


def box_mean(x, w):
    n = x.shape[1]
    xf = x.astype(F32)
    cs = jnp.concatenate([jnp.zeros_like(xf[:, :1]), jnp.cumsum(xf, axis=1)], axis=1)
    pos = jnp.arange(n)
    lo = jnp.clip(pos - w // 2, 0, n)
    hi = jnp.clip(pos + (w - w // 2), 0, n)
    cnt = (hi - lo).astype(F32)
    return (cs[:, hi] - cs[:, lo]) / cnt[None, :, None]


def pool_branch(pp, pool_w, pool_scale, grid):
    b, l, _ = pp.shape
    outs = []
    for gi, w in enumerate(POOL_WINDOWS):
        seg = pp[..., gi * POOL_GW:(gi + 1) * POOL_GW]
        if grid:
            rows = l // GRID_W
            s = box_mean(seg.reshape(b * rows, GRID_W, POOL_GW), w)
            s = box_mean(s.reshape(b, rows, GRID_W * POOL_GW), w)
            s = s.reshape(b, l, POOL_GW)
        else:
            s = box_mean(seg, w)
        outs.append(s - seg.astype(F32))
    pooled = jnp.stack(outs, axis=2)
    y = jnp.einsum('blgc,gcd->blgd', pooled, pool_w.astype(F32))
    return y.reshape(b, l, POOL_W) * pool_scale.astype(F32)


def gmlp_branch(uv, ln_g, ln_b, ws, bs):
    b, l, _ = uv.shape
    act = jax.nn.gelu(uv.astype(F32))
    u, v = act[..., :GMLP_W], act[..., GMLP_W:]
    mu = jnp.mean(v, axis=-1, keepdims=True)
    var = jnp.mean(jnp.square(v - mu), axis=-1, keepdims=True)
    v = (v - mu) * lax.rsqrt(var + LN_EPS) * ln_g.astype(F32) + ln_b.astype(F32)
    vc = v.reshape(b, l // GMLP_CHUNK, GMLP_CHUNK, GMLP_GROUPS, GMLP_GW)
    mixed = jnp.einsum('gij,bnjgc->bnigc', ws.astype(F32), vc) + jnp.transpose(bs.astype(F32))[:, :, None]
    return u * mixed.reshape(b, l, GMLP_W)


def mix_stream(proj, xs, y_scan, d_skip, ssd_g, pool_w, pool_scale, ln_g, ln_b, ws, bs,
               w_br_ssd, w_br_pool, w_br_gmlp, w_o, grid):
    b, l, _ = proj.shape
    z = proj[..., OFF_Z:OFF_XBC].astype(F32)
    y = y_scan + d_skip.astype(F32)[:, None] * xs.astype(F32)
    y_ssd = rmsnorm(y.reshape(b, l, SSD_INNER) * jax.nn.silu(z), ssd_g, SSD_NORM_EPS)
    y_pool = pool_branch(proj[..., OFF_POOL:OFF_GMLP], pool_w, pool_scale, grid)
    y_gmlp = gmlp_branch(proj[..., OFF_GMLP:OFF_GATE], ln_g, ln_b, ws, bs)
    gates = jax.nn.sigmoid(proj[..., OFF_GATE:].astype(F32)).reshape(b, l, N_BRANCH, D_MODEL)
    merged = (gates[..., 0, :] * (y_ssd @ w_br_ssd.astype(F32))
              + gates[..., 1, :] * (y_pool @ w_br_pool.astype(F32))
              + gates[..., 2, :] * (y_gmlp @ w_br_gmlp.astype(F32)))
    return merged @ w_o.astype(F32)


def moe(h, w_rg, b_rg, w_re, b_re, w_e_in, w_e_out):
    b, l, _ = h.shape
    lg = jnp.einsum('bld,dg->blg', h, w_rg).astype(F32) + b_rg.astype(F32)
    pg_top, g_idx = lax.top_k(jax.nn.softmax(lg, axis=-1), 1)
    le = (jnp.einsum('bld,de->ble', h, w_re).astype(F32) + b_re.astype(F32)).reshape(
        b, l, MOE_GROUPS, EXPERTS_PER_GROUP)
    le_sel = jnp.sum(le * jax.nn.one_hot(g_idx[..., 0], MOE_GROUPS, dtype=F32)[..., None], axis=2)
    ve, e_idx = lax.top_k(le_sel, MOE_TOP_K)
    weight = pg_top * jax.nn.softmax(ve, axis=-1)
    expert_id = g_idx * EXPERTS_PER_GROUP + e_idx
    dense_w = jnp.sum(jax.nn.one_hot(expert_id, N_EXPERTS, dtype=F32) * weight[..., None], axis=2)
    out = jnp.zeros((b, l, D_MODEL), F32)
    for e in range(N_EXPERTS):
        gu = h @ w_e_in[e]
        hid = jax.nn.silu(gu[..., :D_EXPERT]) * gu[..., D_EXPERT:]
        out = out + dense_w[..., e:e + 1] * (hid @ w_e_out[e]).astype(F32)
    return out


def setup_inputs(seed: int = 0) -> dict:
    key = jax.random.key(seed)
    ks = list(jax.random.split(key, 40))

    def nrm(i, shape, scale):
        return jax.random.normal(ks[i], shape, F32) * scale

    dt0 = jnp.exp(jax.random.uniform(ks[30], (DEPTH, 2, SSD_HEADS), F32)
                  * (math.log(0.1) - math.log(0.001)) + math.log(0.001))
    return {
        'x': nrm(0, (BATCH, SEQ, D_MODEL), 1.0),
        'c': nrm(1, (BATCH, D_MODEL), 1.0),
        'ctx': nrm(2, (BATCH, CTX_LEN, D_MODEL), 1.0),
        'c_ctx': nrm(3, (D_MODEL,), 1.0),
        'w_mod': nrm(4, (DEPTH, D_MODEL, 6 * D_MODEL), 0.5 * D_MODEL ** -0.5),
        'b_mod': nrm(5, (DEPTH, 6 * D_MODEL), 0.02),
        'g_norm1': 1.0 + nrm(6, (DEPTH, D_MODEL), 0.02),
        'g_norm2': 1.0 + nrm(7, (DEPTH, D_MODEL), 0.02),
        'w_in': nrm(8, (DEPTH, D_MODEL, PROJ_W), D_MODEL ** -0.5),
        'conv_w': nrm(9, (DEPTH, D_CONV, XBC_W), D_CONV ** -0.5),
        'conv_b': nrm(10, (DEPTH, XBC_W), 0.02),
        'dt_bias': dt0 + jnp.log(-jnp.expm1(-dt0)),
        'a_log': jnp.log(jax.random.uniform(ks[31], (DEPTH, 2, SSD_HEADS), F32, 1.0, 16.0)),
        'd_skip': 1.0 + nrm(11, (DEPTH, SSD_HEADS), 0.1),
        'ssd_norm_g': 1.0 + nrm(12, (DEPTH, SSD_INNER), 0.02),
        'pool_w': nrm(13, (DEPTH, POOL_GROUPS, POOL_GW, POOL_GW), POOL_GW ** -0.5),
        'pool_scale': 1.0 + nrm(14, (DEPTH, POOL_W), 0.1),
        'gmlp_ln_g': 1.0 + nrm(15, (DEPTH, GMLP_W), 0.02),
        'gmlp_ln_b': nrm(16, (DEPTH, GMLP_W), 0.02),
        'gmlp_ws': nrm(17, (DEPTH, GMLP_GROUPS, GMLP_CHUNK, GMLP_CHUNK), GMLP_CHUNK ** -0.5),
        'gmlp_bs': 1.0 + nrm(18, (DEPTH, GMLP_GROUPS, GMLP_CHUNK), 0.02),
        'w_br_ssd': nrm(19, (DEPTH, SSD_INNER, D_MODEL), SSD_INNER ** -0.5),
        'w_br_pool': nrm(20, (DEPTH, POOL_W, D_MODEL), POOL_W ** -0.5),
        'w_br_gmlp': nrm(21, (DEPTH, GMLP_W, D_MODEL), GMLP_W ** -0.5),
        'w_o': nrm(22, (DEPTH, D_MODEL, D_MODEL), D_MODEL ** -0.5),
        'w_rg': nrm(23, (DEPTH, D_MODEL, MOE_GROUPS), D_MODEL ** -0.5),
        'b_rg': nrm(24, (DEPTH, MOE_GROUPS), 0.01),
        'w_re': nrm(25, (DEPTH, D_MODEL, N_EXPERTS), D_MODEL ** -0.5),
        'b_re': nrm(26, (DEPTH, N_EXPERTS), 0.01),
        'w_e_in': nrm(27, (DEPTH, N_EXPERTS, D_MODEL, 2 * D_EXPERT), D_MODEL ** -0.5),
        'w_e_out': nrm(28, (DEPTH, N_EXPERTS, D_EXPERT, D_MODEL), D_EXPERT ** -0.5),
        'g_final': 1.0 + nrm(29, (D_MODEL,), 0.02),
    }


def reference(x, c, ctx, c_ctx, w_mod, b_mod, g_norm1, g_norm2, w_in, conv_w, conv_b,
              dt_bias, a_log, d_skip, ssd_norm_g, pool_w, pool_scale, gmlp_ln_g, gmlp_ln_b,
              gmlp_ws, gmlp_bs, w_br_ssd, w_br_pool, w_br_gmlp, w_o, w_rg, b_rg, w_re, b_re,
              w_e_in, w_e_out, g_final):
    silu_c = jax.nn.silu(c)
    silu_cc = jax.nn.silu(c_ctx)
    for i in range(DEPTH):
        last = i == DEPTH - 1
        mod = silu_c @ w_mod[i] + b_mod[i]
        mod_c = silu_cc @ w_mod[i] + b_mod[i]
        sh1, sc1, g1, sh2, sc2, g2 = jnp.split(mod, 6, axis=-1)
        csh1, csc1, cg1, csh2, csc2, cg2 = jnp.split(mod_c, 6, axis=-1)
        a = -jnp.exp(a_log[i].astype(F32))

        h = modulated_norm(x, g_norm1[i], sh1[:, None], sc1[:, None])
        hc = modulated_norm(ctx, g_norm1[i], csh1, csc1)
        proj = h @ w_in[i]
        if last:
            ssd_cols_c = hc @ w_in[i][:, OFF_XBC:OFF_POOL]
        else:
            proj_c = hc @ w_in[i]
            ssd_cols_c = proj_c[..., OFF_XBC:OFF_POOL]

        xs_c, bm_c, cm_c, dt_c = ssd_inputs(ssd_cols_c, conv_w[i], conv_b[i], dt_bias[i])
        h0 = jnp.zeros((xs_c.shape[0], SSD_HEADS, SSD_HEAD_DIM, SSD_STATE), F32)
        y_c, hf_c, hb_c = ssd_bidir(xs_c, bm_c, cm_c, dt_c, a, h0, h0)
        xs, bm, cm, dt = ssd_inputs(proj[..., OFF_XBC:OFF_POOL], conv_w[i], conv_b[i], dt_bias[i])
        y_l, _, _ = ssd_bidir(xs, bm, cm, dt, a, hf_c, hb_c)

        mix = mix_stream(proj, xs, y_l, d_skip[i], ssd_norm_g[i], pool_w[i], pool_scale[i],
                         gmlp_ln_g[i], gmlp_ln_b[i], gmlp_ws[i], gmlp_bs[i],
                         w_br_ssd[i], w_br_pool[i], w_br_gmlp[i], w_o[i], True)
        x = x + (g1[:, None].astype(F32) * mix).astype(x.dtype)
        h2 = modulated_norm(x, g_norm2[i], sh2[:, None], sc2[:, None])
        x = x + (g2[:, None].astype(F32) * moe(h2, w_rg[i], b_rg[i], w_re[i], b_re[i],
                                                w_e_in[i], w_e_out[i])).astype(x.dtype)

        if not last:
            mix_c = mix_stream(proj_c, xs_c, y_c, d_skip[i], ssd_norm_g[i], pool_w[i],
                               pool_scale[i], gmlp_ln_g[i], gmlp_ln_b[i], gmlp_ws[i],
                               gmlp_bs[i], w_br_ssd[i], w_br_pool[i], w_br_gmlp[i], w_o[i], False)
            ctx = ctx + (cg1.astype(F32) * mix_c).astype(ctx.dtype)
            hc2 = modulated_norm(ctx, g_norm2[i], csh2, csc2)
            ctx = ctx + (cg2.astype(F32) * moe(hc2, w_rg[i], b_rg[i], w_re[i], b_re[i],
                                              w_e_in[i], w_e_out[i])).astype(ctx.dtype)
    return rmsnorm(x, g_final).astype(x.dtype)
```

```python
import contextlib
import numpy as np
import concourse.bass as bass
import concourse.mybir as mybir
from concourse.bass_utils import run_bass_kernel_spmd

F32 = mybir.dt.float32
AF = mybir.ActivationFunctionType
ALU = mybir.AluOpType
AX = mybir.AxisListType

D = 1024
DEPTH = 4
SEQ = 16384
CTX = 256
NCORE = 8
PROJ_W = 7712
OFF_XBC, OFF_DT, OFF_POOL, OFF_GMLP, OFF_GATE = 1024, 3072, 3104, 3616, 4640
POOL_WINDOWS = (2, 4, 8, 16)
RMS_EPS, SSD_EPS, LN_EPS = 1e-6, 1e-5, 1e-5
NT = 33
NCH = 130
SEM_R = 30000


class S:
    def __init__(self, ap, key):
        self.ap, self.key = ap, key


READ_KW = ("in_", "in0", "in1", "lhsT", "rhs", "scalar", "scalar1", "scalar2", "bias", "scale",
           "identity", "data0", "data1", "initial")
WRITE_KW = ("out", "accum_out", "ap")
ENGS = ("tensor", "vector", "scalar", "gpsimd", "sync")


class Prog:
    def __init__(self, nc):
        self.nc = nc
        self.ops = {e: [] for e in ENGS}
        self.cnt = {e: 0 for e in ENGS}
        self.dcnt = {e: 0 for e in ENGS}
        self.lastw = {}
        self.readers = {}
        self.seen = {e: {} for e in ENGS}
        self.K = 6
        self.semkeys = []
        self.stack = contextlib.ExitStack()
        self.npsum = 0

    def sb(self, name, shape, dt=F32):
        return self.stack.enter_context(self.nc.sbuf_tensor(name, list(shape), dt))

    def ps(self, name, shape=(128, 512), dt=F32):
        return self.stack.enter_context(self.nc.psum_tensor(name, list(shape), dt))

    def _need(self, eng, tok, waits):
        if tok is None:
            return
        key, val = tok
        if eng == "tensor" and key[0] == "c" and key[1] == "tensor":
            return
        if self.seen[eng].get(key, 0) >= val:
            return
        self.seen[eng][key] = val
        waits.append(tok)

    def _regions(self, kw):
        reads, writes = [], []
        clean = {}
        for k, v in kw.items():
            key = None
            if isinstance(v, S):
                key, v = v.key, v.ap
            if isinstance(v, bass.AP):
                if key is None:
                    key = v.tensor.name
                if k in WRITE_KW:
                    writes.append(key)
                elif k in READ_KW:
                    reads.append(key)
            clean[k] = v
        return clean, reads, writes

    def op(self, eng, name, **kw):
        extra_r = kw.pop("_r", ())
        extra_w = kw.pop("_w", ())
        kw, reads, writes = self._regions(kw)
        reads = list(reads) + list(extra_r)
        writes = list(writes) + list(extra_w)
        is_dma = name == "dma_start"
        waits = []
        for r in reads:
            self._need(eng, self.lastw.get(r), waits)
        for r in writes:
            self._need(eng, self.lastw.get(r), waits)
            for k2, v2 in self.readers.get(r, {}).items():
                self._need(eng, (k2, v2), waits)
        if is_dma:
            i = self.dcnt[eng]
            self.dcnt[eng] += 1
            key = ("d", eng, i % self.K)
            val = 16 * (i // self.K + 1)
            if i >= self.K:
                self._need(eng, (key, val - 16), waits)
            inc = 16
        else:
            self.cnt[eng] += 1
            n = self.cnt[eng] - 1
            key = ("c", eng, n // SEM_R)
            val = n % SEM_R + 1
            inc = 1
        tok = (key, val)
        if key not in self.semkeys:
            self.semkeys.append(key)
        for r in writes:
            self.lastw[r] = tok
            self.readers[r] = {}
        for r in reads:
            d = self.readers.setdefault(r, {})
            if d.get(key, 0) < val:
                d[key] = val
        self.ops[eng].append((name, kw, waits, key, inc))
        return tok

    def finish(self):
        waits = []
        for e in ENGS:
            n = self.dcnt[e]
            for i in range(max(0, n - self.K), n):
                self._need("sync", (("d", e, i % self.K), 16 * (i // self.K + 1)), waits)
        for e in ENGS:
            if self.cnt[e]:
                n = self.cnt[e] - 1
                self._need("sync", (("c", e, n // SEM_R), n % SEM_R + 1), waits)
        self.ops["sync"].append(("__wait__", {}, waits, None, 0))

    def emit(self):
        nc = self.nc
        sems = {}
        for i, key in enumerate(self.semkeys):
            sems[key] = self.stack.enter_context(nc.semaphore("s%d" % i))
        block = self.stack.enter_context(nc.Block())
        ops = self.ops

        def run(e, lst):
            for name, kw, waits, key, inc in lst:
                for (k2, v2) in waits:
                    e.wait_ge(sems[k2], v2)
                if name == "__wait__":
                    continue
                ins = getattr(e, name)(**kw)
                ins.then_inc(sems[key], inc)

        if ops["sync"]:
            @block.sync
            def _(e):
                run(e, ops["sync"])
        if ops["tensor"]:
            @block.tensor
            def _(e):
                run(e, ops["tensor"])
        if ops["vector"]:
            @block.vector
            def _(e):
                run(e, ops["vector"])
        if ops["scalar"]:
            @block.scalar
            def _(e):
                run(e, ops["scalar"])
        if ops["gpsimd"]:
            @block.gpsimd
            def _(e):
                run(e, ops["gpsimd"])
        self.stack.close()


def dram_in(nc, name, shape):
    return nc.dram_tensor(name, list(shape), F32, kind="ExternalInput").ap()


def dram_out(nc, name, shape):
    return nc.dram_tensor(name, list(shape), F32, kind="ExternalOutput").ap()


def emit_mod(p, cT, wmod, bmodb, ncol, ps_tiles, name, CWM=256):
    sc = p.sb(name + "_sc", [128, 2, 8])
    p.op("sync", "dma_start", out=sc[:], in_=cT)
    p.op("scalar", "activation", out=sc[:], in_=sc[:], func=AF.Silu)
    lh = p.sb(name + "_lh", [128, 16, 128])
    for r in range(2):
        for k in range(8):
            p.op("vector", "tensor_copy", out=S(lh[:, r * 8 + k, :], name + "_lh%d" % (r * 8 + k)),
                 in_=sc[:, r, k:k + 1].broadcast_to([128, 128]))
    bm = p.sb(name + "_bm", [128, ncol])
    p.op("sync", "dma_start", out=bm[:], in_=bmodb)
    outs = [p.sb(name + "_m%d" % r, [128, ncol]) for r in range(2)]
    wv = wmod.rearrange("(k q) n -> q k n", q=128)
    wbuf = [p.sb(name + "_w%d" % i, [128, 8, CWM]) for i in range(2)]
    for ci in range(ncol // CWM):
        wb = wbuf[ci % 2]
        p.op("sync", "dma_start", out=wb[:], in_=wv[:, :, ci * CWM:(ci + 1) * CWM])
        for r in range(2):
            pt = ps_tiles[(ci * 2 + r) % len(ps_tiles)]
            for k in range(8):
                p.op("tensor", "matmul", out=pt[:, 0:CWM], lhsT=S(lh[:, r * 8 + k, :], name + "_lh%d" % (r * 8 + k)),
                     rhs=wb[:, k, :], start=(k == 0), stop=(k == 7))
            p.op("vector", "tensor_tensor", out=outs[r][:, ci * CWM:(ci + 1) * CWM],
                 in0=pt[:, 0:CWM], in1=bm[:, ci * CWM:(ci + 1) * CWM], op=ALU.add)
    return outs


def emit_rstd(p, src, ss, junk, eps, n):
    p.op("scalar", "activation", out=junk, in_=src, func=AF.Square, accum_out=ss)
    p.op("vector", "tensor_scalar", out=ss, in0=ss, scalar1=1.0 / n, scalar2=eps, op0=ALU.mult, op1=ALU.add)
    p.op("scalar", "activation", out=ss, in_=ss, func=AF.Sqrt)
    p.op("vector", "reciprocal", out=ss, in_=ss)


def emit_transpose(p, src, nblk, dstT, ident, ps_tiles, eng_cycle=("scalar", "vector"), key=None, skey=None):
    for q in range((nblk + 3) // 4):
        pt = ps_tiles[q % len(ps_tiles)]
        nb = min(4, nblk - q * 4)
        for j in range(nb):
            jj = q * 4 + j
            si = src[:, jj * 128:(jj + 1) * 128]
            p.op("tensor", "transpose", out=pt[:, j * 128:(j + 1) * 128], in_=(S(si, skey) if skey else si),
                 identity=ident[:, :])
        e = eng_cycle[q % len(eng_cycle)]
        o = dstT[:, q * 4:q * 4 + nb, :]
        if key:
            o = S(o, key)
        i = pt[:, 0:nb * 128].rearrange("q (j t) -> q j t", j=nb)
        if e == "scalar":
            p.op("scalar", "activation", out=o, in_=i, func=AF.Copy)
        else:
            p.op("vector", "tensor_copy", out=o, in_=i)


def build_l1():
    nc = bass.Bass("TRN2", target_bir_lowering=False)
    xin = dram_in(nc, "xin", [NT * 128, D])
    cT = dram_in(nc, "cT", [128, 2, 8])
    wmod = dram_in(nc, "wmod", [D, 2048])
    bmodb = dram_in(nc, "bmodb", [128, 2048])
    gnb = dram_in(nc, "gnb", [128, D])
    w_in = dram_in(nc, "w_in", [D, PROJ_W])
    identd = dram_in(nc, "ident", [128, 128])
    proj = dram_out(nc, "proj", [NT * 128, PROJ_W])
    p = Prog(nc)
    ident = p.sb("identsb", [128, 128])
    p.op("sync", "dma_start", out=ident[:], in_=identd)
    pst = [p.ps("ps%d" % i) for i in range(8)]
    mods = emit_mod(p, cT, wmod, bmodb, 2048, pst[0:4], "mod")
    gn = p.sb("gn", [128, D])
    p.op("sync", "dma_start", out=gn[:], in_=gnb)
    gmul = [p.sb("gmul%d" % r, [128, D]) for r in range(2)]
    for r in range(2):
        p.op("vector", "scalar_tensor_tensor", out=gmul[r][:], in0=mods[r][:, D:2 * D], scalar=1.0, in1=gn[:],
             op0=ALU.add, op1=ALU.mult)
    HALF = 17
    hT = p.sb("hT", [128, HALF, 8, 128])
    xt = [p.sb("xt%d" % i, [128, D]) for i in range(2)]
    hh = [p.sb("hh%d" % i, [128, D]) for i in range(2)]
    ss = [p.sb("ss%d" % i, [128, 1]) for i in range(2)]
    NB = 16
    CW = PROJ_W // NB
    wv = w_in.rearrange("(k q) n -> q k n", q=128)
    wb = [p.sb("wb%d" % i, [128, 8, CW]) for i in range(2)]
    ob = [p.sb("ob%d" % i, [128, CW]) for i in range(4)]
    it = 0
    wi = 0
    for t0 in range(0, NT, HALF):
        tiles = list(range(t0, min(NT, t0 + HALF)))
        for t in tiles:
            r = 1 if t == NT - 1 else 0
            b = t % 2
            p.op("sync", "dma_start", out=xt[b][:], in_=xin[t * 128:(t + 1) * 128, :])
            emit_rstd(p, xt[b][:], ss[b][:], hh[b][:], RMS_EPS, D)
            p.op("vector", "scalar_tensor_tensor", out=hh[b][:], in0=xt[b][:], scalar=ss[b][:, 0:1], in1=gmul[r][:],
                 op0=ALU.mult, op1=ALU.mult)
            p.op("vector", "tensor_tensor", out=hh[b][:], in0=hh[b][:], in1=mods[r][:, 0:D], op=ALU.add)
            emit_transpose(p, hh[b], 8, hT[:, t - t0, :, :], ident, pst[0:4], key="hT%d" % (t - t0))
        for cb in range(NB):
            w = wb[wi % 2]
            wi += 1
            p.op("gpsimd", "dma_start", out=w[:], in_=wv[:, :, cb * CW:(cb + 1) * CW])
            for t in tiles:
                pt = pst[4 + it % 4]
                o = ob[it % 4]
                for k in range(8):
                    p.op("tensor", "matmul", out=pt[:, 0:CW], lhsT=S(hT[:, t - t0, k, :], "hT%d" % (t - t0)),
                         rhs=w[:, k, :], start=(k == 0), stop=(k == 7))
                if it % 2 == 0:
                    p.op("scalar", "activation", out=o[:], in_=pt[:, 0:CW], func=AF.Copy)
                else:
                    p.op("vector", "tensor_copy", out=o[:], in_=pt[:, 0:CW])
                p.op("sync", "dma_start", out=proj[t * 128:(t + 1) * 128, cb * CW:(cb + 1) * CW], in_=o[:])
                it += 1
    p.finish()
    p.emit()
    return nc


def build_l2():
    nc = bass.Bass("TRN2", target_bir_lowering=False)
    xbc_c = dram_in(nc, "xbc_c", [512, CTX + 4])
    xbc_l = dram_in(nc, "xbc_l", [512, SEQ + 4])
    dtr = dram_in(nc, "dtr", [NCH * 128, 8])
    ppd = dram_in(nc, "pp", [NCH * 128, 128])
    rcd = dram_in(nc, "rc", [128, NCH])
    convwd = dram_in(nc, "convw", [128, 4, 5])
    convbd = dram_in(nc, "convb", [128, 4])
    dtbd = dram_in(nc, "dtb", [128, 8])
    alogd = dram_in(nc, "alog", [128, 8])
    dskipd = dram_in(nc, "dskip", [128, 4])
    poolwd = dram_in(nc, "poolw", [128, 128])
    pscaled = dram_in(nc, "pscale", [128, 128])
    cstd = dram_in(nc, "cst", [8, 128, 128])
    pm2d = dram_in(nc, "pm2", [9, 128, 128])
    pm1d = dram_in(nc, "pm1", [3, 128, 128])
    youts = [dram_out(nc, "yf", [NCH * 128, 256]), dram_out(nc, "yb", [NCH * 128, 256])]
    ypool = dram_out(nc, "ypool", [NCH * 128, 128])
    p = Prog(nc)
    cst = p.sb("csts", [128, 8, 128])
    p.op("sync", "dma_start", out=cst[:], in_=cstd.rearrange("c q n -> q c n"))
    pm2 = p.sb("pm2s", [128, 9, 128])
    p.op("sync", "dma_start", out=pm2[:], in_=pm2d.rearrange("c q n -> q c n"))
    pm1 = p.sb("pm1s", [128, 3, 128])
    p.op("sync", "dma_start", out=pm1[:], in_=pm1d.rearrange("c q n -> q c n"))
    IDENT, ONES = cst[:, 0, :], cst[:, 1, :]
    TRI = [cst[:, 2, :], cst[:, 3, :]]
    SL = [cst[:, 4, :], cst[:, 5, :]]
    MASK = [cst[:, 6, :], cst[:, 7, :]]
    small = {}
    for nm, dd, sh in (("convw", convwd, [128, 4, 5]), ("convb", convbd, [128, 4]), ("dtb", dtbd, [128, 8]),
                       ("alog", alogd, [128, 8]), ("dskip", dskipd, [128, 4]), ("poolw", poolwd, [128, 128]),
                       ("pscale", pscaled, [128, 128]), ("rc", rcd, [128, NCH])):
        small[nm] = p.sb(nm + "s", sh)
        p.op("sync", "dma_start", out=small[nm][:], in_=dd)
    convw, convb, dskip, poolw, pscale, rc = (small[k] for k in ("convw", "convb", "dskip", "poolw", "pscale", "rc"))
    pp = p.sb("ppall", [128, NCH, 128])
    ppv = ppd.rearrange("(t q) c -> q t c", q=128)
    for t0 in range(0, NCH, 13):
        p.op("gpsimd", "dma_start", out=S(pp[:, t0:t0 + 13, :], "pp%d" % (t0 // 13)), in_=ppv[:, t0:t0 + 13, :])
    dta = p.sb("dta", [128, NCH, 8])
    dA = p.sb("dA", [128, NCH, 8])
    tmpa = p.sb("tmpa", [128, NCH, 8])
    dtv = dtr.rearrange("(t q) c -> q t c", q=128)
    for t0 in range(0, NCH, 13):
        p.op("gpsimd", "dma_start", out=dta[:, t0:t0 + 13, :], in_=dtv[:, t0:t0 + 13, :])
    bc8 = lambda a: a[:, :].unsqueeze(1).broadcast_to([128, NCH, 8])
    p.op("vector", "tensor_tensor", out=dta[:], in0=dta[:], in1=bc8(small["dtb"]), op=ALU.add)
    p.op("scalar", "activation", out=tmpa[:], in_=dta[:], func=AF.Abs)
    p.op("scalar", "activation", out=tmpa[:], in_=tmpa[:], func=AF.Exp, scale=-1.0)
    p.op("scalar", "activation", out=tmpa[:], in_=tmpa[:], func=AF.Ln, bias=1.0)
    p.op("vector", "tensor_scalar", out=dta[:], in0=dta[:], scalar1=0.0, scalar2=None, op0=ALU.max)
    p.op("vector", "tensor_tensor", out=dta[:], in0=dta[:], in1=tmpa[:], op=ALU.add)
    aneg = p.sb("aneg", [128, 8])
    p.op("scalar", "activation", out=aneg[:], in_=small["alog"][:], func=AF.Exp)
    p.op("vector", "tensor_scalar", out=aneg[:], in0=aneg[:], scalar1=-1.0, scalar2=None, op0=ALU.mult)
    p.op("vector", "tensor_tensor", out=dA[:], in0=dta[:], in1=bc8(aneg), op=ALU.mult)

    hst = [p.sb("hst%d" % d, [128, 256]) for d in range(2)]
    for d in range(2):
        p.op("vector", "memset", ap=hst[d][:], constant=0.0)
    NS = 2
    xr = [p.sb("xr%d" % i, [128, 4, 132]) for i in range(NS)]
    xc = [p.sb("xc%d" % i, [128, 4, 128]) for i in range(NS)]
    tok = [p.sb("tok%d" % i, [128, 384]) for i in range(NS)]
    sm = [p.sb("sm%d" % i, [128, 32]) for i in range(NS)]
    xdt = [p.sb("xdt%d" % i, [128, 256]) for i in range(NS)]
    xdtw = [p.sb("xdtw%d" % i, [128, 256]) for i in range(NS)]
    cbm = [p.sb("cbm%d" % i, [128, 128]) for i in range(NS)]
    Lh = [p.sb("Lh%d" % i, [128, 4, 128]) for i in range(NS)]
    Eb = [p.sb("Eb%d" % i, [128, 4, 128]) for i in range(NS)]
    MT = [p.sb("MT%d" % i, [128, 4, 128]) for i in range(NS)]
    ysb = [p.sb("ysb%d" % i, [128, 256]) for i in range(NS)]
    ytmp = [p.sb("ytmp%d" % i, [128, 256]) for i in range(NS)]
    pmt = [p.sb("pmt%d" % i, [128, 128]) for i in range(NS)]
    pmT = [p.sb("pmT%d" % i, [128, 128]) for i in range(NS)]
    ypb = [p.sb("ypb%d" % i, [128, 128]) for i in range(NS)]
    psT, psS, psC, psE, psY, psO, psH, psP = (p.ps("ps%d" % i) for i in range(8))
    h4 = lambda a: a.rearrange("q (h e) -> q h e", h=4)
    b64 = lambda a: a.unsqueeze(2).broadcast_to([128, 4, 64])

    def pool_tile(ti, s):
        if ti < 2:
            PM, rad, lo, hi = pm1, 1, 0, 2
        else:
            PM, rad, lo, hi = pm2, 4, 2, NCH
        ds = [dl for dl in range(-rad, rad + 1) if lo <= ti + dl < hi]
        for n, dl in enumerate(ds):
            tj = ti + dl
            p.op("tensor", "matmul", out=psP[:, 0:128], lhsT=PM[:, dl + rad, :], rhs=S(pp[:, tj, :], "pp%d" % (tj // 13)),
                 start=(n == 0), stop=(n == len(ds) - 1))
        p.op("vector", "scalar_tensor_tensor", out=pmt[s][:], in0=psP[:, 0:128], scalar=rc[:, ti:ti + 1],
             in1=S(pp[:, ti, :], "pp%d" % (ti // 13)), op0=ALU.mult, op1=ALU.subtract)
        p.op("tensor", "transpose", out=psP[:, 128:256], in_=pmt[s][:], identity=IDENT)
        p.op("scalar", "activation", out=pmT[s][:], in_=psP[:, 128:256], func=AF.Copy)
        p.op("tensor", "matmul", out=psP[:, 256:384], lhsT=pmT[s][:], rhs=poolw[:], start=True, stop=True)
        p.op("vector", "tensor_tensor", out=ypb[s][:], in0=psP[:, 256:384], in1=pscale[:], op=ALU.mult)
        p.op("gpsimd", "dma_start", out=ypool[ti * 128:(ti + 1) * 128, :], in_=ypb[s][:])

    def chunk(ci, d, s):
        src, lc = (xbc_c, ci) if ci < 2 else (xbc_l, ci - 2)
        p.op("sync", "dma_start", out=xr[s][:], in_=src.rearrange("(j q) t -> q j t", q=128)[:, :, lc * 128:lc * 128 + 132])
        for j in range(4):
            p.op("vector", "tensor_scalar", out=xc[s][:, j, :], in0=xr[s][:, j, 0:128], scalar1=convw[:, j, 0:1],
                 scalar2=None, op0=ALU.mult)
            for k in range(1, 5):
                p.op("vector", "scalar_tensor_tensor", out=xc[s][:, j, :], in0=xr[s][:, j, k:k + 128],
                     scalar=convw[:, j, k:k + 1], in1=xc[s][:, j, :], op0=ALU.mult, op1=ALU.add)
            p.op("scalar", "activation", out=xc[s][:, j, :], in_=xc[s][:, j, :], func=AF.Silu, bias=convb[:, j:j + 1])
        for j in range(3):
            p.op("tensor", "transpose", out=psT[:, j * 128:(j + 1) * 128], in_=xc[s][:, j, :], identity=IDENT)
        p.op("scalar", "activation", out=tok[s][:], in_=psT[:, 0:384], func=AF.Copy)
        dA4 = dA[:, ci, d * 4:(d + 1) * 4]
        dt4 = dta[:, ci, d * 4:(d + 1) * 4]
        p.op("tensor", "matmul", out=psS[:, 0:4], lhsT=TRI[d], rhs=dA4, start=True, stop=True)
        p.op("tensor", "matmul", out=psS[:, 8:12], lhsT=ONES, rhs=dA4, start=True, stop=True)
        m = sm[s]
        p.op("vector", "tensor_copy", out=m[:, 0:4], in_=psS[:, 0:4])
        p.op("vector", "tensor_copy", out=m[:, 8:12], in_=psS[:, 8:12])
        p.op("vector", "tensor_tensor", out=m[:, 12:16], in0=m[:, 8:12], in1=m[:, 0:4], op=ALU.subtract)
        p.op("scalar", "activation", out=m[:, 12:16], in_=m[:, 12:16], func=AF.Exp)
        p.op("scalar", "activation", out=m[:, 16:20], in_=m[:, 0:4], func=AF.Exp)
        p.op("scalar", "activation", out=m[:, 20:24], in_=m[:, 8:12], func=AF.Exp)
        p.op("vector", "tensor_tensor", out=m[:, 24:28], in0=dt4, in1=m[:, 12:16], op=ALU.mult)
        xs4 = h4(tok[s][:, 0:256])
        p.op("vector", "tensor_tensor", out=h4(xdt[s][:, :]), in0=xs4, in1=b64(dt4), op=ALU.mult)
        p.op("vector", "tensor_tensor", out=h4(xdtw[s][:, :]), in0=xs4, in1=b64(m[:, 24:28]), op=ALU.mult)
        p.op("tensor", "matmul", out=psC[:, 0:128], lhsT=xc[s][:, 2, :], rhs=xc[s][:, 3, :], start=True, stop=True)
        p.op("vector", "tensor_tensor", out=cbm[s][:], in0=psC[:, 0:128], in1=MASK[d], op=ALU.mult)
        for h in range(4):
            p.op("vector", "tensor_scalar", out=Lh[s][:, h, :], in0=SL[d], scalar1=dA4[:, h:h + 1], scalar2=None,
                 op0=ALU.mult)
        for h in range(4):
            p.op("tensor", "matmul", out=psE[:, h * 128:(h + 1) * 128], lhsT=Lh[s][:, h, :], rhs=TRI[d],
                 start=True, stop=True)
        p.op("scalar", "activation", out=Eb[s][:], in_=psE[:, :].rearrange("q (h e) -> q h e", h=4), func=AF.Exp)
        p.op("vector", "tensor_tensor", out=MT[s][:], in0=Eb[s][:], in1=cbm[s][:, :].unsqueeze(1).broadcast_to([128, 4, 128]),
             op=ALU.mult)
        for h in range(4):
            p.op("tensor", "matmul", out=psY[:, h * 64:(h + 1) * 64], lhsT=MT[s][:, h, :], rhs=xdt[s][:, h * 64:(h + 1) * 64],
                 start=True, stop=True)
        p.op("tensor", "matmul", out=psO[:, 0:256], lhsT=xc[s][:, 3, :], rhs=hst[d][:], start=True, stop=True)
        p.op("vector", "tensor_tensor", out=h4(ysb[s][:, :]), in0=h4(psO[:, 0:256]), in1=b64(m[:, 16:20]), op=ALU.mult)
        p.op("vector", "tensor_tensor", out=ysb[s][:], in0=ysb[s][:], in1=psY[:, 0:256], op=ALU.add)
        if d == 0:
            p.op("vector", "tensor_tensor", out=h4(ytmp[s][:, :]), in0=xs4, in1=b64(dskip[:, :]), op=ALU.mult)
            p.op("vector", "tensor_tensor", out=ysb[s][:], in0=ysb[s][:], in1=ytmp[s][:], op=ALU.add)
        p.op("sync", "dma_start", out=youts[d][ci * 128:(ci + 1) * 128, :], in_=ysb[s][:])
        p.op("tensor", "matmul", out=psH[:, 0:256], lhsT=tok[s][:, 256:384], rhs=xdtw[s][:], start=True, stop=True)
        p.op("vector", "tensor_tensor", out=h4(hst[d][:, :]), in0=h4(hst[d][:, :]), in1=b64(m[:, 20:24]), op=ALU.mult)
        p.op("vector", "tensor_tensor", out=hst[d][:], in0=hst[d][:], in1=psH[:, 0:256], op=ALU.add)

    it = 0
    for ci in range(NCH):
        chunk(ci, 0, it % NS)
        pool_tile(ci, it % NS)
        it += 1
    for ci in [1, 0] + list(range(NCH - 1, 1, -1)):
        chunk(ci, 1, it % NS)
        it += 1
    p.finish()
    p.emit()
    return nc


def build_l3a():
    nc = bass.Bass("TRN2", target_bir_lowering=False)
    din = lambda n, sh: dram_in(nc, n, sh)
    xd, zd, yfd, ybd = (din(n, [NT * 128, D]) for n in ("x", "z", "yf", "yb"))
    ypd = din("yp", [NT * 128, 512])
    uvd = din("uv", [NT * 128, D])
    gtd = din("gates", [NT * 128, 3 * D])
    cT = din("cT", [128, 2, 8])
    wmod = din("wmod", [D, D])
    bmodb = din("bmodb", [128, D])
    ssdgd, lngd, lnbd = din("ssdg", [128, D]), din("lng", [128, 512]), din("lnb", [128, 512])
    wsTd, bsTd = din("wsT", [4, 128, 128]), din("bsT", [128, 4])
    wbsd, wbpd, wbgd, wod = din("wbs", [D, D]), din("wbp", [512, D]), din("wbg", [512, D]), din("wo", [D, D])
    identd = din("ident", [128, 128])
    xmid = dram_out(nc, "xmid", [NT * 128, D])
    p = Prog(nc)
    ident = p.sb("identsb", [128, 128])
    p.op("sync", "dma_start", out=ident[:], in_=identd)
    pst = [p.ps("ps%d" % i) for i in range(8)]
    psA, psB, psC, psT = pst[0:2], pst[2:4], pst[4:6], pst[6:8]
    g1 = emit_mod(p, cT, wmod, bmodb, D, pst[0:4], "mod")
    def load(name, dd, shape, view=None, eng="sync"):
        t = p.sb(name, shape)
        p.op(eng, "dma_start", out=t[:], in_=(view if view is not None else dd))
        return t
    ssdg, lng, lnb = load("ssdgs", ssdgd, [128, D]), load("lngs", lngd, [128, 512]), load("lnbs", lnbd, [128, 512])
    wsT = load("wsTs", wsTd, [128, 4, 128], wsTd.rearrange("g q n -> q g n"))
    bsT = load("bsTs", bsTd, [128, 4])
    wbs = load("wbss", wbsd, [128, 8, D], wbsd.rearrange("(k q) n -> q k n", q=128), "gpsimd")
    wbp = load("wbps", wbpd, [128, 4, D], wbpd.rearrange("(k q) n -> q k n", q=128), "gpsimd")
    wbg = load("wbgs", wbgd, [128, 4, D], wbgd.rearrange("(k q) n -> q k n", q=128), "gpsimd")
    wo = load("wos", wod, [128, 8, D], wod.rearrange("(k q) n -> q k n", q=128), "gpsimd")
    xt, zt, yft, ybt, uvt = (p.sb(n, [128, D]) for n in ("xt", "zt", "yft", "ybt", "uvt"))
    ypt = p.sb("ypt", [128, 512])
    gtt = p.sb("gtt", [128, 3 * D])
    t1 = p.sb("t1", [128, D])
    mg = p.sb("mg", [128, D])
    vn = p.sb("vn", [128, 512])
    gm = p.sb("gm", [128, 512])
    TT = p.sb("TT", [128, 8, 128])
    ss = p.sb("ss", [128, 1])
    st6 = p.sb("st6", [128, 6])
    mv = p.sb("mv", [128, 2])
    H = lambda a, h: a[:, h * 512:(h + 1) * 512]
    for t in range(NT):
        r = 1 if t == NT - 1 else 0
        rows = slice(t * 128, (t + 1) * 128)
        for i, (tt_, dd_) in enumerate(((zt, zd), (yft, yfd), (ybt, ybd), (uvt, uvd), (gtt, gtd), (xt, xd), (ypt, ypd))):
            p.op("sync" if i % 2 == 0 else "gpsimd", "dma_start", out=tt_[:], in_=dd_[rows, :])
        p.op("vector", "tensor_tensor", out=yft[:], in0=yft[:], in1=ybt[:], op=ALU.add)
        p.op("scalar", "activation", out=zt[:], in_=zt[:], func=AF.Silu)
        p.op("vector", "tensor_tensor", out=t1[:], in0=yft[:], in1=zt[:], op=ALU.mult)
        emit_rstd(p, t1[:], ss[:], zt[:], SSD_EPS, D)
        p.op("vector", "scalar_tensor_tensor", out=t1[:], in0=t1[:], scalar=ss[:, 0:1], in1=ssdg[:], op0=ALU.mult, op1=ALU.mult)
        emit_transpose(p, t1, 8, TT[:, :, :], ident, psT)
        for h in range(2):
            for k in range(8):
                p.op("tensor", "matmul", out=psA[h][:, :], lhsT=TT[:, k, :], rhs=wbs[:, k, h * 512:(h + 1) * 512],
                     start=(k == 0), stop=(k == 7))
        p.op("scalar", "activation", out=uvt[:], in_=uvt[:], func=AF.Gelu_apprx_tanh)
        p.op("vector", "bn_stats", out=st6[:], in_=uvt[:, 512:1024])
        p.op("vector", "bn_aggr", out=mv[:], in_=st6[:])
        p.op("vector", "tensor_scalar", out=mv[:, 1:2], in0=mv[:, 1:2], scalar1=LN_EPS, scalar2=None, op0=ALU.add)
        p.op("scalar", "activation", out=mv[:, 1:2], in_=mv[:, 1:2], func=AF.Sqrt)
        p.op("vector", "reciprocal", out=mv[:, 1:2], in_=mv[:, 1:2])
        p.op("vector", "tensor_scalar", out=vn[:], in0=uvt[:, 512:1024], scalar1=mv[:, 0:1], scalar2=mv[:, 1:2],
             op0=ALU.subtract, op1=ALU.mult)
        p.op("vector", "tensor_tensor", out=vn[:], in0=vn[:], in1=lng[:], op=ALU.mult)
        p.op("vector", "tensor_tensor", out=vn[:], in0=vn[:], in1=lnb[:], op=ALU.add)
        for g in range(4):
            p.op("tensor", "matmul", out=psB[0][:, g * 128:(g + 1) * 128], lhsT=wsT[:, g, :], rhs=vn[:, g * 128:(g + 1) * 128],
                 start=True, stop=True)
        for g in range(4):
            gs = slice(g * 128, (g + 1) * 128)
            p.op("vector", "scalar_tensor_tensor", out=gm[:, gs], in0=psB[0][:, gs], scalar=bsT[:, g:g + 1], in1=uvt[:, gs],
                 op0=ALU.add, op1=ALU.mult)
        emit_transpose(p, gm, 4, TT[:, 0:4, :], ident, psT)
        for h in range(2):
            for k in range(4):
                p.op("tensor", "matmul", out=psB[h][:, :], lhsT=TT[:, k, :], rhs=wbg[:, k, h * 512:(h + 1) * 512],
                     start=(k == 0), stop=(k == 3))
        emit_transpose(p, ypt, 4, TT[:, 4:8, :], ident, psT)
        for h in range(2):
            for k in range(4):
                p.op("tensor", "matmul", out=psC[h][:, :], lhsT=TT[:, 4 + k, :], rhs=wbp[:, k, h * 512:(h + 1) * 512],
                     start=(k == 0), stop=(k == 3))
        p.op("scalar", "activation", out=gtt[:], in_=gtt[:], func=AF.Sigmoid)
        for h in range(2):
            p.op("vector", "tensor_tensor", out=H(mg, h), in0=H(gtt, h), in1=psA[h][:, :], op=ALU.mult)
            p.op("vector", "tensor_tensor", out=H(t1, h), in0=H(gtt, 2 + h), in1=psC[h][:, :], op=ALU.mult)
            p.op("vector", "tensor_tensor", out=H(mg, h), in0=H(mg, h), in1=H(t1, h), op=ALU.add)
            p.op("vector", "tensor_tensor", out=H(t1, h), in0=H(gtt, 4 + h), in1=psB[h][:, :], op=ALU.mult)
            p.op("vector", "tensor_tensor", out=H(mg, h), in0=H(mg, h), in1=H(t1, h), op=ALU.add)
        emit_transpose(p, mg, 8, TT[:, :, :], ident, psT)
        for h in range(2):
            for k in range(8):
                p.op("tensor", "matmul", out=psA[h][:, :], lhsT=TT[:, k, :], rhs=wo[:, k, h * 512:(h + 1) * 512],
                     start=(k == 0), stop=(k == 7))
            p.op("vector", "tensor_tensor", out=H(t1, h), in0=psA[h][:, :], in1=H(g1[r], h), op=ALU.mult)
            p.op("vector", "tensor_tensor", out=H(xt, h), in0=H(xt, h), in1=H(t1, h), op=ALU.add)
        p.op("sync", "dma_start", out=xmid[rows, :], in_=xt[:])
    p.finish()
    p.emit()
    return nc


def build_l3b(final):
    nc = bass.Bass("TRN2", target_bir_lowering=False)
    din = lambda n, sh: dram_in(nc, n, sh)
    xd = din("x", [NT * 128, D])
    cT = din("cT", [128, 2, 8])
    wmod = din("wmod", [D, 3 * D])
    bmodb = din("bmodb", [128, 3 * D])
    gn2d = din("gn2", [128, D])
    wrd, brd = din("wr", [D, 20]), din("brb", [128, 20])
    weind, weoutd = din("wein", [16, D, D]), din("weout", [16, 512, D])
    gfd = din("gfin", [128, D])
    identd = din("ident", [128, 128])
    xout = dram_out(nc, "xout", [NT * 128, D])
    p = Prog(nc)
    ident = p.sb("identsb", [128, 128])
    p.op("sync", "dma_start", out=ident[:], in_=identd)
    pst = [p.ps("ps%d" % i) for i in range(8)]
    mods = emit_mod(p, cT, wmod, bmodb, 3 * D, pst[0:4], "mod")
    gn2 = p.sb("gn2s", [128, D])
    p.op("sync", "dma_start", out=gn2[:], in_=gn2d)
    for r in range(2):
        p.op("vector", "scalar_tensor_tensor", out=mods[r][:, D:2 * D], in0=mods[r][:, D:2 * D], scalar=1.0, in1=gn2[:],
             op0=ALU.add, op1=ALU.mult)
    gf = gn2
    if final:
        p.op("sync", "dma_start", out=gf[:], in_=gfd)
    wr = p.sb("wrs", [128, 8, 20])
    p.op("sync", "dma_start", out=wr[:], in_=wrd.rearrange("(k q) n -> q k n", q=128))
    brb = p.sb("brbs", [128, 20])
    p.op("sync", "dma_start", out=brb[:], in_=brd)
    wein = p.sb("weins", [128, 8, D])
    weout = p.sb("weouts", [128, 4, D])
    G = 4
    xg = p.sb("xg", [128, G, D])
    acc = p.sb("acc", [128, G, D])
    h2T = p.sb("h2T", [128, 8, G * 128])
    hidT = p.sb("hidT", [128, 4, G * 128])
    sgb = [p.sb("sgb%d" % i, [128, G * 128]) for i in range(2)]
    h2b = p.sb("h2b", [128, D])
    dw = p.sb("dw", [128, G, 16])
    ss = p.sb("ss", [128, 1])
    rt = p.sb("rt", [128, 64])
    psGU, psO, psT, psR = pst[0:4], pst[4:6], [pst[6]], pst[7]
    weinv = weind.rearrange("e (k q) n -> e q k n", q=128)
    weoutv = weoutd.rearrange("e (k q) n -> e q k n", q=128)
    for g0 in range(0, NT, G):
        tiles = list(range(g0, min(NT, g0 + G)))
        T = len(tiles) * 128
        for tt, t in enumerate(tiles):
            r = 1 if t == NT - 1 else 0
            xk = "xg%d" % tt
            X = lambda: S(xg[:, tt, :], xk)
            p.op("sync", "dma_start", out=X(), in_=xd[t * 128:(t + 1) * 128, :])
            p.op("scalar", "activation", out=h2b[:], in_=X(), func=AF.Square, accum_out=ss[:])
            p.op("vector", "tensor_scalar", out=ss[:], in0=ss[:], scalar1=1.0 / D, scalar2=RMS_EPS, op0=ALU.mult, op1=ALU.add)
            p.op("scalar", "activation", out=ss[:], in_=ss[:], func=AF.Sqrt)
            p.op("vector", "reciprocal", out=ss[:], in_=ss[:])
            p.op("vector", "scalar_tensor_tensor", out=h2b[:], in0=X(), scalar=ss[:, 0:1], in1=mods[r][:, D:2 * D],
                 op0=ALU.mult, op1=ALU.mult)
            p.op("vector", "tensor_tensor", out=h2b[:], in0=h2b[:], in1=mods[r][:, 0:D], op=ALU.add)
            emit_transpose(p, h2b, 8, h2T[:, :, tt * 128:(tt + 1) * 128], ident, psT, key="h2T%d" % tt)
            for k in range(8):
                p.op("tensor", "matmul", out=psR[:, 0:20], lhsT=S(h2T[:, k, tt * 128:(tt + 1) * 128], "h2T%d" % tt),
                     rhs=wr[:, k, :], start=(k == 0), stop=(k == 7))
            c = lambda a, b: rt[:, a:b]
            lg, le = c(0, 4), c(4, 20)
            p.op("vector", "tensor_tensor", out=c(0, 20), in0=psR[:, 0:20], in1=brb[:], op=ALU.add)
            p.op("vector", "reduce_max", out=c(20, 21), in_=lg, axis=AX.X)
            p.op("vector", "tensor_scalar", out=c(24, 28), in0=lg, scalar1=c(20, 21), scalar2=None, op0=ALU.is_equal)
            p.op("vector", "tensor_scalar", out=c(21, 22), in0=c(20, 21), scalar1=-1.0, scalar2=None, op0=ALU.mult)
            p.op("scalar", "activation", out=c(28, 32), in_=lg, func=AF.Exp, bias=c(21, 22), accum_out=c(22, 23))
            p.op("vector", "reciprocal", out=c(23, 24), in_=c(22, 23))
            p.op("vector", "tensor_scalar", out=c(32, 36), in0=c(4, 8), scalar1=c(24, 25), scalar2=None, op0=ALU.mult)
            for g in range(1, 4):
                p.op("vector", "scalar_tensor_tensor", out=c(32, 36), in0=c(4 + 4 * g, 8 + 4 * g), scalar=c(24 + g, 25 + g),
                     in1=c(32, 36), op0=ALU.mult, op1=ALU.add)
            p.op("vector", "reduce_max", out=c(36, 37), in_=c(32, 36), axis=AX.X)
            p.op("vector", "tensor_scalar", out=c(40, 44), in0=c(32, 36), scalar1=c(36, 37), scalar2=None, op0=ALU.is_equal)
            p.op("vector", "scalar_tensor_tensor", out=c(44, 48), in0=c(40, 44), scalar=-1e30, in1=c(32, 36),
                 op0=ALU.mult, op1=ALU.add)
            p.op("vector", "reduce_max", out=c(37, 38), in_=c(44, 48), axis=AX.X)
            p.op("vector", "tensor_scalar", out=c(48, 52), in0=c(44, 48), scalar1=c(37, 38), scalar2=None, op0=ALU.is_equal)
            p.op("vector", "tensor_tensor", out=c(38, 39), in0=c(37, 38), in1=c(36, 37), op=ALU.subtract)
            p.op("scalar", "activation", out=c(38, 39), in_=c(38, 39), func=AF.Exp)
            p.op("vector", "tensor_scalar", out=c(39, 40), in0=c(38, 39), scalar1=1.0, scalar2=None, op0=ALU.add)
            p.op("vector", "reciprocal", out=c(39, 40), in_=c(39, 40))
            p.op("vector", "tensor_tensor", out=c(38, 39), in0=c(38, 39), in1=c(39, 40), op=ALU.mult)
            p.op("vector", "tensor_tensor", out=c(39, 40), in0=c(39, 40), in1=c(23, 24), op=ALU.mult)
            p.op("vector", "tensor_tensor", out=c(38, 39), in0=c(38, 39), in1=c(23, 24), op=ALU.mult)
            p.op("vector", "tensor_scalar", out=c(52, 56), in0=c(40, 44), scalar1=c(39, 40), scalar2=None, op0=ALU.mult)
            p.op("vector", "scalar_tensor_tensor", out=c(52, 56), in0=c(48, 52), scalar=c(38, 39), in1=c(52, 56),
                 op0=ALU.mult, op1=ALU.add)
            for g in range(4):
                p.op("vector", "tensor_scalar", out=dw[:, tt, 4 * g:4 * g + 4], in0=c(52, 56), scalar1=c(24 + g, 25 + g),
                     scalar2=None, op0=ALU.mult)
        for e in range(16):
            for k in range(8):
                p.op("sync", "dma_start", out=S(wein[:, k, :], "wein%d" % k), in_=weinv[e, :, k, :])
            for k in range(4):
                p.op("gpsimd", "dma_start", out=S(weout[:, k, :], "weout%d" % k), in_=weoutv[e, :, k, :])
            for j in range(4):
                pg_, pu_ = psGU[(j % 2) * 2], psGU[(j % 2) * 2 + 1]
                for (pt, off) in ((pg_, 0), (pu_, 512)):
                    for k in range(8):
                        p.op("tensor", "matmul", out=pt[:, 0:T], lhsT=S(wein[:, k, off + j * 128:off + (j + 1) * 128], "wein%d" % k),
                             rhs=h2T[:, k, 0:T], start=(k == 0), stop=(k == 7), _r=["h2T%d" % q for q in range(len(tiles))])
                sb_ = sgb[j % 2]
                p.op("scalar", "activation", out=sb_[:, 0:T], in_=pg_[:, 0:T], func=AF.Silu)
                p.op("vector", "tensor_tensor", out=S(hidT[:, j, 0:T], "hidT%d" % j), in0=sb_[:, 0:T], in1=pu_[:, 0:T], op=ALU.mult)
            for tt, t in enumerate(tiles):
                for h in range(2):
                    po = psO[h]
                    for j in range(4):
                        p.op("tensor", "matmul", out=po[:, :], lhsT=S(hidT[:, j, tt * 128:(tt + 1) * 128], "hidT%d" % j),
                             rhs=S(weout[:, j, h * 512:(h + 1) * 512], "weout%d" % j), start=(j == 0), stop=(j == 3))
                    a = S(acc[:, tt, h * 512:(h + 1) * 512], "acc%d_%d" % (tt, h))
                    if e == 0:
                        p.op("vector", "tensor_scalar", out=a, in0=po[:, :], scalar1=dw[:, tt, e:e + 1], scalar2=None, op0=ALU.mult)
                    else:
                        p.op("vector", "scalar_tensor_tensor", out=a, in0=po[:, :], scalar=dw[:, tt, e:e + 1], in1=a,
                             op0=ALU.mult, op1=ALU.add)
        for tt, t in enumerate(tiles):
            r = 1 if t == NT - 1 else 0
            xk = "xg%d" % tt
            for h in range(2):
                a = S(acc[:, tt, h * 512:(h + 1) * 512], "acc%d_%d" % (tt, h))
                p.op("vector", "tensor_tensor", out=a, in0=a, in1=mods[r][:, 2 * D + h * 512:2 * D + (h + 1) * 512], op=ALU.mult)
                p.op("vector", "tensor_tensor", out=S(xg[:, tt, h * 512:(h + 1) * 512], xk), in0=S(xg[:, tt, h * 512:(h + 1) * 512], xk),
                     in1=a, op=ALU.add)
            if final:
                p.op("scalar", "activation", out=h2b[:], in_=S(xg[:, tt, :], xk), func=AF.Square, accum_out=ss[:])
                p.op("vector", "tensor_scalar", out=ss[:], in0=ss[:], scalar1=1.0 / D, scalar2=RMS_EPS, op0=ALU.mult, op1=ALU.add)
                p.op("scalar", "activation", out=ss[:], in_=ss[:], func=AF.Sqrt)
                p.op("vector", "reciprocal", out=ss[:], in_=ss[:])
                p.op("vector", "scalar_tensor_tensor", out=S(xg[:, tt, :], xk), in0=S(xg[:, tt, :], xk), scalar=ss[:, 0:1],
                     in1=gf[:], op0=ALU.mult, op1=ALU.mult)
            p.op("sync", "dma_start", out=xout[t * 128:(t + 1) * 128, :], in_=S(xg[:, tt, :], xk))
    p.finish()
    p.emit()
    return nc


def rep(v, n=128):
    v = np.asarray(v, np.float32).reshape(-1)
    return np.ascontiguousarray(np.broadcast_to(v[None, :], (n, v.shape[0])))
def scan_consts():
    k = np.arange(128)[:, None]; i = np.arange(128)[None, :]
    c = np.zeros((8, 128, 128), np.float32)
    c[0] = np.eye(128); c[1] = 1.0
    c[2] = (k <= i); c[3] = (k >= i); c[4] = (k > i); c[5] = (k < i); c[6] = (i >= k); c[7] = (i <= k)
    return c
def pool_consts(w):
    lo, hi = w // 2, w - w // 2
    pm2 = np.zeros((9, 128, 128), np.float32)
    k = np.arange(128); i = np.arange(128)
    for dl in range(-4, 5):
        rk = 2 * dl + k // 64; ck = k % 64
        ri = i // 64; ci = i % 64
        m = ((rk[:, None] >= ri[None, :] - lo) & (rk[:, None] < ri[None, :] + hi) &
             (ck[:, None] >= ci[None, :] - lo) & (ck[:, None] < ci[None, :] + hi))
        pm2[dl + 4] = m
    pm1 = np.zeros((3, 128, 128), np.float32)
    for dl in range(-1, 2):
        pk = dl * 128 + k
        m = (pk[:, None] >= i[None, :] - lo) & (pk[:, None] < i[None, :] + hi)
        pm1[dl + 1] = m
    def cnt(n):
        pos = np.arange(n)
        return (np.clip(pos + hi, 0, n) - np.clip(pos - lo, 0, n)).astype(np.float32)
    c64, c256 = cnt(64), cnt(256)
    rows = np.arange(16384) // 64; cols = np.arange(16384) % 64
    rc_lat = 1.0 / (c256[rows] * c64[cols])
    rc_ctx = 1.0 / cnt(256)
    rc = np.concatenate([rc_ctx, rc_lat]).astype(np.float32)
    rcT = np.ascontiguousarray(rc.reshape(130, 128).T)
    return pm2, pm1, rcT
def l2_inputs(proj_b, proj_cb, g, W, i):
    xcols = np.r_[OFF_XBC + g * 256: OFF_XBC + (g + 1) * 256, OFF_XBC + 1024 + g * 128: OFF_XBC + 1024 + (g + 1) * 128,
                  OFF_XBC + 1536 + g * 128: OFF_XBC + 1536 + (g + 1) * 128]
    def fm(pr):
        a = np.zeros((512, pr.shape[0] + 4), np.float32)
        a[:, 2:-2] = pr[:, xcols].T
        return a
    dcols = np.r_[OFF_DT + 4 * g: OFF_DT + 4 * g + 4, OFF_DT + 16 + 4 * g: OFF_DT + 16 + 4 * g + 4]
    dtr = np.concatenate([proj_cb[:, dcols], proj_b[:, dcols]], 0)
    pcols = slice(OFF_POOL + g * 128, OFF_POOL + (g + 1) * 128)
    pp = np.concatenate([proj_cb[:, pcols], proj_b[:, pcols]], 0)
    pm2, pm1, rcT = pool_consts(POOL_WINDOWS[g])
    ch = xcols - OFF_XBC
    cw = W['conv_w'][i][:, ch]
    convw = np.ascontiguousarray(cw.T.reshape(4, 128, 5).transpose(1, 0, 2))
    convb = np.ascontiguousarray(W['conv_b'][i][ch].reshape(4, 128).T)
    hs = slice(4 * g, 4 * g + 4)
    return dict(xbc_c=fm(proj_cb), xbc_l=fm(proj_b), dtr=np.ascontiguousarray(dtr), pp=np.ascontiguousarray(pp), rc=rcT,
                convw=convw, convb=convb,
                dtb=rep(np.concatenate([W['dt_bias'][i][0, hs], W['dt_bias'][i][1, hs]])),
                alog=rep(np.concatenate([W['a_log'][i][0, hs], W['a_log'][i][1, hs]])),
                dskip=rep(W['d_skip'][i][hs]), poolw=np.ascontiguousarray(W['pool_w'][i][g]),
                pscale=rep(W['pool_scale'][i][g * 128:(g + 1) * 128]), cst=scan_consts(), pm2=pm2, pm1=pm1)

def tok_shard(lat, ctxa):
    C = lat.shape[-1]
    out = []
    for k in range(8):
        b, q = k // 4, k % 4
        a = np.zeros((NT * 128, C), np.float32)
        a[:4096] = lat[b, q * 4096:(q + 1) * 4096]
        if k < 4:
            a[4096:] = ctxa[k // 2, (k % 2) * 128:(k % 2 + 1) * 128]
        out.append(a)
    return out
def tok_unshard(parts, C):
    lat = np.zeros((2, 16384, C), np.float32); ctxa = np.zeros((2, 256, C), np.float32)
    for k in range(8):
        b, q = k // 4, k % 4
        lat[b, q * 4096:(q + 1) * 4096] = parts[k][:4096]
        if k < 4:
            ctxa[k // 2, (k % 2) * 128:(k % 2 + 1) * 128] = parts[k][4096:]
    return lat, ctxa
def cT_of(c, c_ctx, b):
    cv = np.stack([c[b], c_ctx], 0).astype(np.float32)
    return np.ascontiguousarray(cv.reshape(2, 8, 128).transpose(2, 0, 1))


_PROGS = {}


def _prog(name):
    if name not in _PROGS:
        _PROGS[name] = {"l1": build_l1, "l2": build_l2, "l3a": build_l3a,
                        "l3b": lambda: build_l3b(False), "l3bf": lambda: build_l3b(True)}[name]()
    return _PROGS[name]


def _run(name, maps):
    res = run_bass_kernel_spmd(_prog(name), maps, core_ids=list(range(NCORE)))
    return res.results


def kernel(**inp):
    W = {k: np.asarray(v, np.float32) for k, v in inp.items()}
    ident = np.eye(128, dtype=np.float32)
    cc = np.ascontiguousarray
    X = tok_shard(W["x"], W["ctx"])
    cTs = [cT_of(W["c"], W["c_ctx"], b) for b in range(2)]
    for i in range(DEPTH):
        wm, bm = W["w_mod"][i], W["b_mod"][i]
        maps = [dict(xin=X[k], cT=cTs[k // 4], wmod=cc(wm[:, :2048]), bmodb=rep(bm[:2048]), gnb=rep(W["g_norm1"][i]),
                     w_in=W["w_in"][i], ident=ident) for k in range(NCORE)]
        r1 = _run("l1", maps)
        proj, projc = tok_unshard([r["proj"] for r in r1], PROJ_W)
        del r1
        maps = [l2_inputs(proj[k // 4], projc[k // 4], k % 4, W, i) for k in range(NCORE)]
        r2 = _run("l2", maps)
        ys = {}
        for nm, wd in (("yf", 256), ("yb", 256), ("ypool", 128)):
            lat = np.zeros((2, SEQ, 4 * wd), np.float32)
            cx = np.zeros((2, CTX, 4 * wd), np.float32)
            for k in range(NCORE):
                b, g = k // 4, k % 4
                cx[b][:, g * wd:(g + 1) * wd] = r2[k][nm][:CTX]
                lat[b][:, g * wd:(g + 1) * wd] = r2[k][nm][CTX:]
            ys[nm] = tok_shard(lat, cx)
        del r2
        sl = lambda a, b: tok_shard(cc(proj[..., a:b]), cc(projc[..., a:b]))
        Z, UV, GT = sl(0, 1024), sl(OFF_GMLP, OFF_GATE), sl(OFF_GATE, PROJ_W)
        del proj, projc
        maps = [dict(x=X[k], z=Z[k], yf=ys["yf"][k], yb=ys["yb"][k], yp=ys["ypool"][k], uv=UV[k], gates=GT[k],
                     cT=cTs[k // 4], wmod=cc(wm[:, 2048:3072]), bmodb=rep(bm[2048:3072]),
                     ssdg=rep(W["ssd_norm_g"][i]), lng=rep(W["gmlp_ln_g"][i]), lnb=rep(W["gmlp_ln_b"][i]),
                     wsT=cc(W["gmlp_ws"][i].transpose(0, 2, 1)), bsT=cc(W["gmlp_bs"][i].T),
                     wbs=W["w_br_ssd"][i], wbp=W["w_br_pool"][i], wbg=W["w_br_gmlp"][i], wo=W["w_o"][i], ident=ident)
                for k in range(NCORE)]
        r3 = _run("l3a", maps)
        del Z, UV, GT, ys
        maps = [dict(x=r3[k]["xmid"], cT=cTs[k // 4], wmod=cc(wm[:, 3072:]), bmodb=rep(bm[3072:]), gn2=rep(W["g_norm2"][i]),
                     wr=cc(np.concatenate([W["w_rg"][i], W["w_re"][i]], 1)),
                     brb=rep(np.concatenate([W["b_rg"][i], W["b_re"][i]])), wein=W["w_e_in"][i], weout=W["w_e_out"][i],
                     gfin=rep(W["g_final"]), ident=ident) for k in range(NCORE)]
        r4 = _run("l3bf" if i == DEPTH - 1 else "l3b", maps)
        X = [r4[k]["xout"] for k in range(NCORE)]
        del r3, r4
    lat, _ = tok_unshard(X, D)
    return lat
```

```python
import contextlib
import numpy as np
import concourse.bass as bass
import concourse.mybir as mybir
from concourse.bass_utils import run_bass_kernel_spmd

F32 = mybir.dt.float32
BF16 = mybir.dt.bfloat16
AF = mybir.ActivationFunctionType
ALU = mybir.AluOpType
AX = mybir.AxisListType

D = 1024
DEPTH = 4
SEQ = 16384
CTX = 256
NCORE = 8
PROJ_W = 7712
OFF_XBC, OFF_DT, OFF_POOL, OFF_GMLP, OFF_GATE = 1024, 3072, 3104, 3616, 4640
POOL_WINDOWS = (2, 4, 8, 16)
RMS_EPS, SSD_EPS, LN_EPS = 1e-6, 1e-5, 1e-5
NTT = 130
NCH = NTT
SEM_R = 14000


class S:
    def __init__(self, ap, key):
        self.ap, self.key = ap, key


class LAP:
    def __init__(self, base, tf=()):
        self.base, self.tf = base, tuple(tf)

    def __getitem__(self, idx):
        return LAP(self.base, self.tf + (("g", idx),))

    def rearrange(self, pat, **kw):
        return LAP(self.base, self.tf + (("r", pat, kw),))

    def resolve(self, i):
        nd = len(self.base.shape)
        ap = self.base[(bass.ds(i, 1),) + (slice(None),) * (nd - 1)]
        n = "abcdefgh"[:nd]
        ap = ap.rearrange("%s -> (%s %s) %s" % (" ".join(n), n[0], n[1], " ".join(n[2:])))
        for t in self.tf:
            ap = ap[t[1]] if t[0] == "g" else ap.rearrange(t[1], **t[2])
        return ap


READ_KW = ("in_", "in0", "in1", "lhsT", "rhs", "scalar", "scalar1", "scalar2", "bias", "scale",
           "identity", "data0", "data1", "initial")
WRITE_KW = ("out", "accum_out", "ap")
ENGS = ("tensor", "vector", "scalar", "gpsimd", "sync")


class Prog:
    def __init__(self, nc):
        self.nc = nc
        self.ops = {e: [] for e in ENGS}
        self.cnt = {e: 0 for e in ENGS}
        self.dcnt = {e: 0 for e in ENGS}
        self.lastw = {}
        self.readers = {}
        self.seen = {e: {} for e in ENGS}
        self.K = 12
        self.semkeys = []
        self.stack = contextlib.ExitStack()
        self.npsum = 0
        self.sems = {}
        self.tag = ''

    def sb(self, name, shape, dt=F32):
        return self.stack.enter_context(self.nc.sbuf_tensor(name, list(shape), dt))

    def ps(self, name, shape=(128, 512), dt=F32):
        return self.stack.enter_context(self.nc.psum_tensor(name, list(shape), dt))

    def _need(self, eng, tok, waits):
        if tok is None:
            return
        key, val = tok
        if eng == "tensor" and key[0] == "c" and key[1] == "tensor":
            return
        if self.seen[eng].get(key, 0) >= val:
            return
        self.seen[eng][key] = val
        waits.append(tok)

    def _regions(self, kw):
        reads, writes = [], []
        clean = {}
        for k, v in kw.items():
            key = None
            if isinstance(v, S):
                key, v = v.key, v.ap
            if isinstance(v, bass.AP):
                if key is None:
                    key = v.tensor.name
                if k in WRITE_KW:
                    writes.append(key)
                elif k in READ_KW:
                    reads.append(key)
            clean[k] = v
        return clean, reads, writes

    def op(self, eng, name, **kw):
        extra_r = kw.pop("_r", ())
        extra_w = kw.pop("_w", ())
        kw, reads, writes = self._regions(kw)
        reads = list(reads) + list(extra_r)
        writes = list(writes) + list(extra_w)
        is_dma = name == "dma_start"
        if is_dma:
            eng = "sync"
        waits = []
        for r in reads:
            self._need(eng, self.lastw.get(r), waits)
        for r in writes:
            self._need(eng, self.lastw.get(r), waits)
            for k2, v2 in self.readers.get(r, {}).items():
                self._need(eng, (k2, v2), waits)
        if is_dma:
            i = self.dcnt[eng]
            self.dcnt[eng] += 1
            key = ("d", eng, i % self.K)
            val = 16 * (i // self.K + 1)
            if i >= self.K:
                self._need(eng, (key, val - 16), waits)
            inc = 16
        else:
            self.cnt[eng] += 1
            n = self.cnt[eng] - 1
            key = ("c", eng, n // SEM_R)
            val = n % SEM_R + 1
            inc = 1
        tok = (key, val)
        if key not in self.semkeys:
            self.semkeys.append(key)
        for r in writes:
            self.lastw[r] = tok
            self.readers[r] = {}
        for r in reads:
            d = self.readers.setdefault(r, {})
            if d.get(key, 0) < val:
                d[key] = val
        self.ops[eng].append((name, kw, waits, key, inc))
        return tok

    def barrier(self):
        toks = []
        for e in ENGS:
            n = self.dcnt[e]
            for i in range(max(0, n - self.K), n):
                toks.append((("d", e, i % self.K), 16 * (i // self.K + 1)))
            if self.cnt[e]:
                n = self.cnt[e] - 1
                toks.append((("c", e, n // SEM_R), n % SEM_R + 1))
        for e in ENGS:
            waits = []
            for t in toks:
                self._need(e, t, waits)
            self.ops[e].append(("__wait__", {}, waits, None, 0))

    def begin_phase(self, tag):
        self.tag = tag
        self.pstack = contextlib.ExitStack()

    def psb(self, name, shape, dt=F32):
        return self.pstack.enter_context(self.nc.sbuf_tensor(self.tag + "_" + name, list(shape), dt))

    def end_phase(self):
        self.barrier()
        self.pstack.close()

    def end_segment(self):
        seg = self.ops
        self.ops = {e: [] for e in ENGS}
        self.cnt = {e: 0 for e in ENGS}
        self.dcnt = {e: 0 for e in ENGS}
        self.lastw, self.readers = {}, {}
        self.seen = {e: {} for e in ENGS}
        return seg

    def emit_program(self, init, body, epi, depth):
        nc = self.nc
        st = self.stack
        sems = {key: st.enter_context(nc.semaphore("s%d" % n)) for n, key in enumerate(self.semkeys)}
        SD, SG, SD2 = (st.enter_context(nc.semaphore(n)) for n in ("hs_done", "hs_go", "hs_ack"))
        MASTER = "sync"

        def run(e, lst, i):
            for name, kw, waits, key, inc in lst:
                for (k2, v2) in waits:
                    e.wait_ge(sems[k2], v2)
                if name == "__wait__":
                    continue
                kw = {k: (v.resolve(i) if isinstance(v, LAP) else v) for k, v in kw.items()}
                getattr(e, name)(**kw).then_inc(sems[key], inc)

        def handshake(e, eng):
            e.sem_inc(SD, 1)
            if eng == MASTER:
                e.wait_ge(SD, len(ENGS))
                for sm in sems.values():
                    e.sem_clear(sm)
                e.sem_clear(SD)
                e.sem_inc(SG, 1)
                e.wait_ge(SD2, len(ENGS) - 1)
                e.sem_clear(SG)
                e.sem_clear(SD2)
            else:
                e.wait_ge(SG, 1)
                e.sem_inc(SD2, 1)

        def stream(e, eng):
            run(e, init[eng], None)
            handshake(e, eng)
            with e.Fori(0, depth) as i:
                run(e, body[eng], i)
                handshake(e, eng)
            run(e, epi[eng], None)

        with nc.Block() as block:
            @block.sync
            def _(e):
                stream(e, "sync")

            @block.tensor
            def _(e):
                stream(e, "tensor")

            @block.vector
            def _(e):
                stream(e, "vector")

            @block.scalar
            def _(e):
                stream(e, "scalar")

            @block.gpsimd
            def _(e):
                stream(e, "gpsimd")
        self.stack.close()


def dram_in(nc, name, shape):
    return nc.dram_tensor(name, list(shape), F32, kind="ExternalInput").ap()


def dram_out(nc, name, shape):
    return nc.dram_tensor(name, list(shape), F32, kind="ExternalOutput").ap()


def emit_mod(p, cT, wmod, bmodb, ncol, ps_tiles, name, CWM=256):
    sc = p.psb(name + "_sc", [128, 2, 8])
    p.op("sync", "dma_start", out=sc[:], in_=cT)
    p.op("scalar", "activation", out=sc[:], in_=sc[:], func=AF.Silu)
    lh = p.psb(name + "_lh", [128, 16, 128])
    for r in range(2):
        for k in range(8):
            p.op("vector", "tensor_copy", out=S(lh[:, r * 8 + k, :], name + "_lh%d" % (r * 8 + k)),
                 in_=sc[:, r, k:k + 1].broadcast_to([128, 128]))
    bm = p.psb(name + "_bm", [128, ncol])
    p.op("sync", "dma_start", out=bm[:], in_=bmodb)
    outs = [p.psb(name + "_m%d" % r, [128, ncol]) for r in range(2)]
    wv = wmod.rearrange("(k q) n -> q k n", q=128)
    wbuf = [p.psb(name + "_w%d" % i, [128, 8, CWM]) for i in range(2)]
    for ci in range(ncol // CWM):
        wb = wbuf[ci % 2]
        p.op("sync", "dma_start", out=wb[:], in_=wv[:, :, ci * CWM:(ci + 1) * CWM])
        for r in range(2):
            pt = ps_tiles[(ci * 2 + r) % len(ps_tiles)]
            for k in range(8):
                p.op("tensor", "matmul", out=pt[:, 0:CWM], lhsT=S(lh[:, r * 8 + k, :], name + "_lh%d" % (r * 8 + k)),
                     rhs=wb[:, k, :], start=(k == 0), stop=(k == 7))
            p.op("vector", "tensor_tensor", out=outs[r][:, ci * CWM:(ci + 1) * CWM],
                 in0=pt[:, 0:CWM], in1=bm[:, ci * CWM:(ci + 1) * CWM], op=ALU.add)
    return outs


def emit_rstd(p, src, ss, junk, eps, n):
    p.op("scalar", "activation", out=junk, in_=src, func=AF.Square, accum_out=ss)
    p.op("vector", "tensor_scalar", out=ss, in0=ss, scalar1=1.0 / n, scalar2=eps, op0=ALU.mult, op1=ALU.add)
    p.op("scalar", "activation", out=ss, in_=ss, func=AF.Sqrt)
    p.op("vector", "reciprocal", out=ss, in_=ss)


def emit_transpose(p, src, nblk, dstT, ident, ps_tiles, eng_cycle=("scalar", "vector"), key=None, skey=None, dst2=None, key2=None):
    for q in range((nblk + 3) // 4):
        pt = ps_tiles[q % len(ps_tiles)]
        nb = min(4, nblk - q * 4)
        for j in range(nb):
            jj = q * 4 + j
            si = src[:, jj * 128:(jj + 1) * 128]
            p.op("tensor", "transpose", out=pt[:, j * 128:(j + 1) * 128], in_=(S(si, skey) if skey else si),
                 identity=ident[:, :])
        e = eng_cycle[q % len(eng_cycle)]
        o = dstT[:, q * 4:q * 4 + nb, :]
        if key:
            o = S(o, key)
        i = pt[:, 0:nb * 128].rearrange("q (j t) -> q j t", j=nb)
        if e == "scalar":
            p.op("scalar", "activation", out=o, in_=i, func=AF.Copy)
        else:
            p.op("vector", "tensor_copy", out=o, in_=i)
        if dst2 is not None:
            o2 = dst2[:, q * 4:q * 4 + nb, :]
            if key2:
                o2 = S(o2, key2)
            if e == "scalar":
                p.op("vector", "tensor_copy", out=o2, in_=i)
            else:
                p.op("scalar", "activation", out=o2, in_=i, func=AF.Copy)


def phase_l1(p, R):
    p.begin_phase("l1")
    pst, ident = R["pst"], R["ident"]
    W = R["W"]
    mods = emit_mod(p, R["cT"], W["wmod"][:, 0:2048], W["bmodb"][:, 0:2048], 2048, pst[0:4], "mod")
    gn = p.psb("gn", [128, D])
    p.op("sync", "dma_start", out=gn[:], in_=W["gn1b"])
    gmul = [p.psb("gmul%d" % r, [128, D]) for r in range(2)]
    for r in range(2):
        p.op("vector", "scalar_tensor_tensor", out=gmul[r][:], in0=mods[r][:, D:2 * D], scalar=1.0, in1=gn[:],
             op0=ALU.add, op1=ALU.mult)
    HALF = 16
    hT = p.psb("hT", [128, HALF, 8, 128])
    xt = [p.psb("xt%d" % j, [128, D]) for j in range(2)]
    hh = [p.psb("hh%d" % j, [128, D]) for j in range(2)]
    ss = [p.psb("ss%d" % j, [128, 1]) for j in range(2)]
    wv = W["w_in"].rearrange("(k q) n -> q k n", q=128)
    wb = [p.psb("wb%d" % j, [128, 8, 512]) for j in range(2)]
    ob = [p.psb("ob%d" % j, [128, 512]) for j in range(4)]
    blocks = ([(R["pz"], 0, j * 512, 512) for j in range(2)] + [(R["pm"], 3072, 3072 + j * 392, 392) for j in range(4)] +
              [(R["pg"], OFF_GATE, OFF_GATE + j * 512, 512) for j in range(6)])
    xin = R["xcur"]
    it = 0
    wi = 0
    tiles_all = list(range(NTT))
    groups = [tiles_all[0:2] + tiles_all[2:2 + HALF - 2]]
    rest = tiles_all[HALF:]
    groups += [rest[j:j + HALF] for j in range(0, len(rest), HALF)]
    for tiles in groups:
        t0 = tiles[0]
        for t in tiles:
            r = 1 if t < 2 else 0
            b = t % 2
            p.op("sync", "dma_start", out=xt[b][:], in_=xin[t * 128:(t + 1) * 128, :])
            emit_rstd(p, xt[b][:], ss[b][:], hh[b][:], RMS_EPS, D)
            p.op("vector", "scalar_tensor_tensor", out=hh[b][:], in0=xt[b][:], scalar=ss[b][:, 0:1], in1=gmul[r][:],
                 op0=ALU.mult, op1=ALU.mult)
            p.op("vector", "tensor_tensor", out=hh[b][:], in0=hh[b][:], in1=mods[r][:, 0:D], op=ALU.add)
            emit_transpose(p, hh[b], 8, hT[:, t - t0, :, :], ident, pst[0:4], key=hT.name + "%d" % (t - t0))
        for (pdst, pbase, c0, cw) in blocks:
            w = wb[wi % 2]
            wi += 1
            p.op("sync", "dma_start", out=w[:, :, 0:cw], in_=wv[:, :, c0:c0 + cw])
            for t in tiles:
                pt = pst[4 + it % 4]
                o = ob[it % 4]
                for k in range(8):
                    p.op("tensor", "matmul", out=pt[:, 0:cw], lhsT=S(hT[:, t - t0, k, :], hT.name + "%d" % (t - t0)),
                         rhs=w[:, k, 0:cw], start=(k == 0), stop=(k == 7))
                if it % 2 == 0:
                    p.op("scalar", "activation", out=o[:, 0:cw], in_=pt[:, 0:cw], func=AF.Copy)
                else:
                    p.op("vector", "tensor_copy", out=o[:, 0:cw], in_=pt[:, 0:cw])
                p.op("sync", "dma_start", out=pdst[t * 128:(t + 1) * 128, c0 - pbase:c0 - pbase + cw], in_=o[:, 0:cw])
                it += 1
        subs = []
        lat = [t for t in tiles if t >= 2]
        if tiles[0] < 2:
            subs.append((R["xbc_c"], 0, [0, 1]))
        for j in range(0, len(lat), 4):
            sub = lat[j:j + 4]
            subs.append((R["xbc_l"], (sub[0] - 2) * 128, sub))
        for cb in range(4):
            w = wb[wi % 2]
            wi += 1
            p.op("sync", "dma_start", out=w[:], in_=wv[:, :, 1024 + cb * 512:1024 + (cb + 1) * 512])
            for cc in range(4):
                for (dst, tok0, sub) in subs:
                    n = len(sub) * 128
                    a = sub[0] - t0
                    pt = pst[4 + it % 4]
                    o = ob[it % 4]
                    for k in range(8):
                        p.op("tensor", "matmul", out=pt[:, 0:n], lhsT=w[:, k, cc * 128:(cc + 1) * 128],
                             rhs=hT[:, a:a + len(sub), k, :], start=(k == 0), stop=(k == 7),
                             _r=[hT.name + "%d" % (a + q) for q in range(len(sub))])
                    if it % 2 == 0:
                        p.op("scalar", "activation", out=o[:, 0:n], in_=pt[:, 0:n], func=AF.Copy)
                    else:
                        p.op("vector", "tensor_copy", out=o[:, 0:n], in_=pt[:, 0:n])
                    row0 = (cb * 4 + cc) * 128
                    p.op("sync", "dma_start", out=dst[row0:row0 + 128, 2 + tok0:2 + tok0 + n], in_=o[:, 0:n])
                    it += 1
    p.end_phase()


def phase_l2(p, R, g):
    p.begin_phase("l2_%d" % g)
    W = R["W"]
    pst = R["pst"]
    cst, IDENT = R["cst"], R["ident"][:, :]
    ONES = cst[:, 1, :]
    TRI = [cst[:, 2, :], cst[:, 3, :]]
    SL = [cst[:, 4, :], cst[:, 5, :]]
    MASK = [cst[:, 6, :], cst[:, 7, :]]
    youts = [R["yf"], R["yb"]]
    ypool = R["ypool"]
    pm2 = p.psb("pm2s", [128, 9, 128])
    p.op("sync", "dma_start", out=pm2[:], in_=W["pm2"][g].rearrange("c q n -> q c n"))
    pm1 = p.psb("pm1s", [128, 3, 128])
    p.op("sync", "dma_start", out=pm1[:], in_=W["pm1"][g].rearrange("c q n -> q c n"))
    small = {}
    for nm, dd, sh in (("convw", W["convw"][g], [128, 4, 5]), ("convb", W["convb"][g], [128, 4]),
                       ("dtb", W["dtb"][g], [128, 8]), ("alog", W["alog"][g], [128, 8]),
                       ("dskip", W["dskip"][g], [128, 4]), ("poolw", W["poolw"][g], [128, 128]),
                       ("pscale", W["pscale"][g], [128, 128]), ("rc", W["rc"][g], [128, NCH])):
        small[nm] = p.psb(nm + "s", sh)
        p.op("sync", "dma_start", out=small[nm][:], in_=dd)
    convw, convb, dskip, poolw, pscale, rc = (small[k] for k in ("convw", "convb", "dskip", "poolw", "pscale", "rc"))
    pp = p.psb("ppall", [128, NCH, 128])
    pv = R["pm"].rearrange("(t q) c -> q t c", q=128)
    for t0 in range(0, NCH, 13):
        p.op("gpsimd", "dma_start", out=S(pp[:, t0:t0 + 13, :], pp.name + "%d" % (t0 // 13)),
             in_=pv[:, t0:t0 + 13, 32 + g * 128:32 + (g + 1) * 128])
    dta = p.psb("dta", [128, NCH, 8])
    dA = p.psb("dA", [128, NCH, 8])
    tmpa = p.psb("tmpa", [128, NCH, 8])
    for t0 in range(0, NCH, 13):
        for d in range(2):
            c0 = 16 * d + 4 * g
            p.op("gpsimd", "dma_start", out=dta[:, t0:t0 + 13, 4 * d:4 * d + 4], in_=pv[:, t0:t0 + 13, c0:c0 + 4])
    bc8 = lambda a: a[:, :].unsqueeze(1).broadcast_to([128, NCH, 8])
    p.op("vector", "tensor_tensor", out=dta[:], in0=dta[:], in1=bc8(small["dtb"]), op=ALU.add)
    p.op("scalar", "activation", out=tmpa[:], in_=dta[:], func=AF.Abs)
    p.op("scalar", "activation", out=tmpa[:], in_=tmpa[:], func=AF.Exp, scale=-1.0)
    p.op("scalar", "activation", out=tmpa[:], in_=tmpa[:], func=AF.Ln, bias=1.0)
    p.op("vector", "tensor_scalar", out=dta[:], in0=dta[:], scalar1=0.0, scalar2=None, op0=ALU.max)
    p.op("vector", "tensor_tensor", out=dta[:], in0=dta[:], in1=tmpa[:], op=ALU.add)
    aneg = p.psb("aneg", [128, 8])
    p.op("scalar", "activation", out=aneg[:], in_=small["alog"][:], func=AF.Exp)
    p.op("vector", "tensor_scalar", out=aneg[:], in0=aneg[:], scalar1=-1.0, scalar2=None, op0=ALU.mult)
    p.op("vector", "tensor_tensor", out=dA[:], in0=dta[:], in1=bc8(aneg), op=ALU.mult)
    hst = [p.psb("hst%d" % d, [128, 256]) for d in range(2)]
    for d in range(2):
        p.op("vector", "memset", ap=hst[d][:], constant=0.0)
    NS = 4
    mk = lambda nm, sh: [p.psb("%s%d" % (nm, j), sh) for j in range(NS)]
    xr, xc, xtmp = mk("xr", [128, 4, 132]), mk("xc", [128, 4, 128]), mk("xtmp", [128, 4, 128])
    tok, sm = mk("tok", [128, 384]), mk("sm", [128, 32])
    xdt, xdtw = mk("xdt", [128, 256]), mk("xdtw", [128, 256])
    cbm, Lh, Eb, MT = mk("cbm", [128, 128]), mk("Lh", [128, 4, 128]), mk("Eb", [128, 4, 128]), mk("MT", [128, 4, 128])
    ysb, ytmp = mk("ysb", [128, 256]), mk("ytmp", [128, 256])
    pmt, pmT, ypb = mk("pmt", [128, 128]), mk("pmT", [128, 128]), mk("ypb", [128, 128])
    class PV:
        def __init__(self, t, c0, key):
            self.t, self.c0, self.key = t, c0, key

        def __call__(self, a_, b_):
            return S(self.t[:, self.c0 + a_:self.c0 + b_], self.key)
    psEd = [PV(pst[0], 0, "psE0"), PV(pst[1], 0, "psE1")]
    psTd = [PV(pst[2], 0, "psT0"), PV(pst[3], 0, "psT1")]
    psCd = [PV(pst[2], 384, "psC0"), PV(pst[3], 384, "psC1")]
    psYd = [PV(pst[4], 0, "psY0"), PV(pst[5], 0, "psY1")]
    psOd = [PV(pst[4], 256, "psO0"), PV(pst[5], 256, "psO1")]
    psHd = [PV(pst[6], 0, "psH0"), PV(pst[6], 256, "psH1")]
    psSd = [PV(pst[7], 0, "psS0"), PV(pst[7], 16, "psS1")]
    psP = PV(pst[7], 128, "psP")
    h4 = lambda a: a.rearrange("q (h e) -> q h e", h=4)
    b64 = lambda a: a.unsqueeze(2).broadcast_to([128, 4, 64])
    wbc = lambda k: convw[:, :, k:k + 1].broadcast_to([128, 4, 128])

    def pool_tile(ti, s):
        if ti < 2:
            PM, rad, lo, hi = pm1, 1, 0, 2
        else:
            PM, rad, lo, hi = pm2, 4, 2, NCH
        ds = [dl for dl in range(-rad, rad + 1) if lo <= ti + dl < hi]
        for n, dl in enumerate(ds):
            tj = ti + dl
            p.op("tensor", "matmul", out=psP(0, 128), lhsT=PM[:, dl + rad, :], rhs=S(pp[:, tj, :], pp.name + "%d" % (tj // 13)),
                 start=(n == 0), stop=(n == len(ds) - 1))
        p.op("vector", "scalar_tensor_tensor", out=pmt[s][:], in0=psP(0, 128), scalar=rc[:, ti:ti + 1],
             in1=S(pp[:, ti, :], pp.name + "%d" % (ti // 13)), op0=ALU.mult, op1=ALU.subtract)
        p.op("tensor", "transpose", out=psP(128, 256), in_=pmt[s][:], identity=IDENT)
        p.op("scalar", "activation", out=pmT[s][:], in_=psP(128, 256), func=AF.Copy)
        p.op("tensor", "matmul", out=psP(256, 384), lhsT=pmT[s][:], rhs=poolw[:], start=True, stop=True)
        p.op("vector", "tensor_tensor", out=ypb[s][:], in0=psP(256, 384), in1=pscale[:], op=ALU.mult)
        p.op("gpsimd", "dma_start", out=ypool[ti * 128:(ti + 1) * 128, g * 128:(g + 1) * 128], in_=ypb[s][:])

    def chunk(ci, d, s):
        psT, psS, psC, psE, psY, psO, psH = psTd[d], psSd[d], psCd[d], psEd[d], psYd[d], psOd[d], psHd[d]
        src, lc = (R["xbc_c"], ci) if ci < 2 else (R["xbc_l"], ci - 2)
        cs = slice(lc * 128, lc * 128 + 132)
        p.op("sync", "dma_start", out=xr[s][:, 0:2, :], in_=src[g * 256:(g + 1) * 256, :].rearrange("(j q) t -> q j t", q=128)[:, :, cs])
        p.op("sync", "dma_start", out=xr[s][:, 2, :], in_=src[1024 + g * 128:1024 + (g + 1) * 128, cs])
        p.op("sync", "dma_start", out=xr[s][:, 3, :], in_=src[1536 + g * 128:1536 + (g + 1) * 128, cs])
        p.op("vector", "tensor_tensor", out=xc[s][:], in0=xr[s][:, :, 0:128], in1=wbc(0), op=ALU.mult)
        for k in range(1, 5):
            p.op("gpsimd" if k % 2 else "vector", "tensor_tensor", out=xtmp[s][:], in0=xr[s][:, :, k:k + 128], in1=wbc(k), op=ALU.mult)
            p.op("vector", "tensor_tensor", out=xc[s][:], in0=xc[s][:], in1=xtmp[s][:], op=ALU.add)
        p.op("vector", "tensor_tensor", out=xc[s][:], in0=xc[s][:], in1=convb[:, :].unsqueeze(2).broadcast_to([128, 4, 128]), op=ALU.add)
        p.op("scalar", "activation", out=xc[s][:], in_=xc[s][:], func=AF.Silu)
        for j in range(3):
            p.op("tensor", "transpose", out=psT(j * 128, (j + 1) * 128), in_=xc[s][:, j, :], identity=IDENT)
        p.op("scalar", "activation", out=tok[s][:], in_=psT(0, 384), func=AF.Copy)
        dA4 = dA[:, ci, d * 4:(d + 1) * 4]
        dt4 = dta[:, ci, d * 4:(d + 1) * 4]
        p.op("tensor", "matmul", out=psS(0, 4), lhsT=TRI[d], rhs=dA4, start=True, stop=True)
        p.op("tensor", "matmul", out=psS(8, 12), lhsT=ONES, rhs=dA4, start=True, stop=True)
        m = sm[s]
        p.op("vector", "tensor_copy", out=m[:, 0:12], in_=psS(0, 12))
        p.op("vector", "tensor_tensor", out=m[:, 12:16], in0=m[:, 8:12], in1=m[:, 0:4], op=ALU.subtract)
        p.op("scalar", "activation", out=m[:, 12:16], in_=m[:, 12:16], func=AF.Exp)
        p.op("scalar", "activation", out=m[:, 16:20], in_=m[:, 0:4], func=AF.Exp)
        p.op("scalar", "activation", out=m[:, 20:24], in_=m[:, 8:12], func=AF.Exp)
        p.op("vector", "tensor_tensor", out=m[:, 24:28], in0=dt4, in1=m[:, 12:16], op=ALU.mult)
        xs4 = h4(tok[s][:, 0:256])
        p.op("gpsimd", "tensor_tensor", out=h4(xdt[s][:, :]), in0=xs4, in1=b64(dt4), op=ALU.mult)
        p.op("gpsimd", "tensor_tensor", out=h4(xdtw[s][:, :]), in0=xs4, in1=b64(m[:, 24:28]), op=ALU.mult)
        p.op("tensor", "matmul", out=psC(0, 128), lhsT=xc[s][:, 2, :], rhs=xc[s][:, 3, :], start=True, stop=True)
        p.op("vector", "tensor_tensor", out=cbm[s][:], in0=psC(0, 128), in1=MASK[d], op=ALU.mult)
        p.op("vector", "tensor_tensor", out=Lh[s][:], in0=SL[d].unsqueeze(1).broadcast_to([128, 4, 128]),
             in1=dA4.unsqueeze(2).broadcast_to([128, 4, 128]), op=ALU.mult)
        for h in range(4):
            p.op("tensor", "matmul", out=psE(h * 128, (h + 1) * 128), lhsT=Lh[s][:, h, :], rhs=TRI[d],
                 start=True, stop=True)
        p.op("scalar", "activation", out=Eb[s][:], in_=S(psE(0, 512).ap.rearrange("q (h e) -> q h e", h=4), psE.key), func=AF.Exp)
        p.op("vector", "tensor_tensor", out=MT[s][:], in0=Eb[s][:], in1=cbm[s][:, :].unsqueeze(1).broadcast_to([128, 4, 128]),
             op=ALU.mult)
        for h in range(4):
            p.op("tensor", "matmul", out=psY(h * 64, (h + 1) * 64), lhsT=MT[s][:, h, :], rhs=xdt[s][:, h * 64:(h + 1) * 64],
                 start=True, stop=True)
        p.op("tensor", "matmul", out=psO(0, 256), lhsT=xc[s][:, 3, :], rhs=hst[d][:], start=True, stop=True)
        p.op("vector", "tensor_tensor", out=h4(ysb[s][:, :]), in0=S(h4(psO(0, 256).ap), psO.key), in1=b64(m[:, 16:20]), op=ALU.mult)
        p.op("vector", "tensor_tensor", out=ysb[s][:], in0=ysb[s][:], in1=psY(0, 256), op=ALU.add)
        if d == 0:
            p.op("gpsimd", "tensor_tensor", out=h4(ytmp[s][:, :]), in0=xs4, in1=b64(dskip[:, :]), op=ALU.mult)
            p.op("vector", "tensor_tensor", out=ysb[s][:], in0=ysb[s][:], in1=ytmp[s][:], op=ALU.add)
        p.op("sync", "dma_start", out=youts[d][ci * 128:(ci + 1) * 128, g * 256:(g + 1) * 256], in_=ysb[s][:])
        p.op("tensor", "matmul", out=psH(0, 256), lhsT=tok[s][:, 256:384], rhs=xdtw[s][:], start=True, stop=True)
        p.op("vector", "tensor_tensor", out=h4(hst[d][:, :]), in0=h4(hst[d][:, :]), in1=b64(m[:, 20:24]), op=ALU.mult)
        p.op("vector", "tensor_tensor", out=hst[d][:], in0=hst[d][:], in1=psH(0, 256), op=ALU.add)

    fwd = list(range(NCH))
    bwd = [1, 0] + list(range(NCH - 1, 1, -1))
    for n in range(NCH):
        chunk(fwd[n], 0, n % 2)
        pool_tile(fwd[n], n % 2)
        chunk(bwd[n], 1, 2 + n % 2)
    p.end_phase()


def phase_l3a(p, R):
    p.begin_phase("l3a")
    W = R["W"]
    pst, ident = R["pst"], R["ident"]
    psA, psB, psC, psT = pst[0:2], pst[2:4], pst[4:6], pst[6:8]
    g1 = emit_mod(p, R["cT"], W["wmod"][:, 2048:3072], W["bmodb"][:, 2048:3072], D, pst[0:4], "mod")

    def load(name, shape, view, eng="sync"):
        t = p.psb(name, shape)
        p.op(eng, "dma_start", out=t[:], in_=view)
        return t
    ssdg, lng, lnb = load("ssdgs", [128, D], W["ssdg"]), load("lngs", [128, 512], W["lng"]), load("lnbs", [128, 512], W["lnb"])
    wsT = load("wsTs", [128, 4, 128], W["wsT"].rearrange("g q n -> q g n"))
    bsT = load("bsTs", [128, 4], W["bsT"])
    kq = lambda a: a.rearrange("(k q) n -> q k n", q=128)
    wbs = load("wbss", [128, 8, D], kq(W["wbs"]))
    wbp = load("wbps", [128, 4, D], kq(W["wbp"]))
    wbg = load("wbgs", [128, 4, D], kq(W["wbg"]))
    wo = load("wos", [128, 8, D], kq(W["wo"]))
    xt, zt, yft, ybt, uvt = (p.psb(n, [128, D]) for n in ("xt", "zt", "yft", "ybt", "uvt"))
    ypt = p.psb("ypt", [128, 512])
    gtt = p.psb("gtt", [128, 3 * D])
    t1, mg = p.psb("t1", [128, D]), p.psb("mg", [128, D])
    vn, gm = p.psb("vn", [128, 512]), p.psb("gm", [128, 512])
    TT = p.psb("TT", [128, 8, 128])
    ss, st6, mv = p.psb("ss", [128, 1]), p.psb("st6", [128, 6]), p.psb("mv", [128, 2])
    H = lambda a, h: a[:, h * 512:(h + 1) * 512]
    for t in range(NTT):
        r = 1 if t < 2 else 0
        rows = slice(t * 128, (t + 1) * 128)
        srcs = ((zt, R["pz"][rows, :]), (yft, R["yf"][rows, :]), (ybt, R["yb"][rows, :]), (uvt, R["pm"][rows, 544:1568]),
                (gtt, R["pg"][rows, :]), (xt, R["xcur"][rows, :]), (ypt, R["ypool"][rows, :]))
        for j, (tt_, dd_) in enumerate(srcs):
            p.op("sync" if j % 2 == 0 else "gpsimd", "dma_start", out=tt_[:], in_=dd_)
        p.op("vector", "tensor_tensor", out=yft[:], in0=yft[:], in1=ybt[:], op=ALU.add)
        p.op("scalar", "activation", out=zt[:], in_=zt[:], func=AF.Silu)
        p.op("vector", "tensor_tensor", out=t1[:], in0=yft[:], in1=zt[:], op=ALU.mult)
        emit_rstd(p, t1[:], ss[:], zt[:], SSD_EPS, D)
        p.op("vector", "scalar_tensor_tensor", out=t1[:], in0=t1[:], scalar=ss[:, 0:1], in1=ssdg[:], op0=ALU.mult, op1=ALU.mult)
        emit_transpose(p, t1, 8, TT[:, :, :], ident, psT)
        for h in range(2):
            for k in range(8):
                p.op("tensor", "matmul", out=psA[h][:, :], lhsT=TT[:, k, :], rhs=wbs[:, k, h * 512:(h + 1) * 512],
                     start=(k == 0), stop=(k == 7))
        p.op("scalar", "activation", out=uvt[:], in_=uvt[:], func=AF.Gelu_apprx_tanh)
        p.op("vector", "bn_stats", out=st6[:], in_=uvt[:, 512:1024])
        p.op("vector", "bn_aggr", out=mv[:], in_=st6[:])
        p.op("vector", "tensor_scalar", out=mv[:, 1:2], in0=mv[:, 1:2], scalar1=LN_EPS, scalar2=None, op0=ALU.add)
        p.op("scalar", "activation", out=mv[:, 1:2], in_=mv[:, 1:2], func=AF.Sqrt)
        p.op("vector", "reciprocal", out=mv[:, 1:2], in_=mv[:, 1:2])
        p.op("vector", "tensor_scalar", out=vn[:], in0=uvt[:, 512:1024], scalar1=mv[:, 0:1], scalar2=mv[:, 1:2],
             op0=ALU.subtract, op1=ALU.mult)
        p.op("vector", "tensor_tensor", out=vn[:], in0=vn[:], in1=lng[:], op=ALU.mult)
        p.op("vector", "tensor_tensor", out=vn[:], in0=vn[:], in1=lnb[:], op=ALU.add)
        for g in range(4):
            p.op("tensor", "matmul", out=psB[0][:, g * 128:(g + 1) * 128], lhsT=wsT[:, g, :], rhs=vn[:, g * 128:(g + 1) * 128],
                 start=True, stop=True)
        for g in range(4):
            gs = slice(g * 128, (g + 1) * 128)
            p.op("vector", "scalar_tensor_tensor", out=gm[:, gs], in0=psB[0][:, gs], scalar=bsT[:, g:g + 1], in1=uvt[:, gs],
                 op0=ALU.add, op1=ALU.mult)
        emit_transpose(p, gm, 4, TT[:, 0:4, :], ident, psT)
        for h in range(2):
            for k in range(4):
                p.op("tensor", "matmul", out=psB[h][:, :], lhsT=TT[:, k, :], rhs=wbg[:, k, h * 512:(h + 1) * 512],
                     start=(k == 0), stop=(k == 3))
        emit_transpose(p, ypt, 4, TT[:, 4:8, :], ident, psT)
        for h in range(2):
            for k in range(4):
                p.op("tensor", "matmul", out=psC[h][:, :], lhsT=TT[:, 4 + k, :], rhs=wbp[:, k, h * 512:(h + 1) * 512],
                     start=(k == 0), stop=(k == 3))
        p.op("scalar", "activation", out=gtt[:], in_=gtt[:], func=AF.Sigmoid)
        for h in range(2):
            p.op("vector", "tensor_tensor", out=H(mg, h), in0=H(gtt, h), in1=psA[h][:, :], op=ALU.mult)
            p.op("vector", "tensor_tensor", out=H(t1, h), in0=H(gtt, 2 + h), in1=psC[h][:, :], op=ALU.mult)
            p.op("vector", "tensor_tensor", out=H(mg, h), in0=H(mg, h), in1=H(t1, h), op=ALU.add)
            p.op("vector", "tensor_tensor", out=H(t1, h), in0=H(gtt, 4 + h), in1=psB[h][:, :], op=ALU.mult)
            p.op("vector", "tensor_tensor", out=H(mg, h), in0=H(mg, h), in1=H(t1, h), op=ALU.add)
        emit_transpose(p, mg, 8, TT[:, :, :], ident, psT)
        for h in range(2):
            for k in range(8):
                p.op("tensor", "matmul", out=psA[h][:, :], lhsT=TT[:, k, :], rhs=wo[:, k, h * 512:(h + 1) * 512],
                     start=(k == 0), stop=(k == 7))
            p.op("vector", "tensor_tensor", out=H(t1, h), in0=psA[h][:, :], in1=H(g1[r], h), op=ALU.mult)
            p.op("vector", "tensor_tensor", out=H(xt, h), in0=H(xt, h), in1=H(t1, h), op=ALU.add)
        p.op("sync", "dma_start", out=R["xmid"][rows, :], in_=xt[:])
    p.end_phase()


def phase_l3b(p, R):
    final = False
    p.begin_phase("l3b")
    W = R["W"]
    pst, ident = R["pst"], R["ident"]
    mods = emit_mod(p, R["cT"], W["wmod"][:, 3072:6144], W["bmodb"][:, 3072:6144], 3 * D, pst[0:4], "mod")
    gn2 = p.psb("gn2s", [128, D])
    p.op("sync", "dma_start", out=gn2[:], in_=W["gn2b"])
    for r in range(2):
        p.op("vector", "scalar_tensor_tensor", out=mods[r][:, D:2 * D], in0=mods[r][:, D:2 * D], scalar=1.0, in1=gn2[:],
             op0=ALU.add, op1=ALU.mult)
    gf = gn2
    if final:
        p.op("sync", "dma_start", out=gf[:], in_=W["gfin"])
    wr = p.psb("wrs", [128, 8, 20])
    p.op("sync", "dma_start", out=wr[:], in_=W["wr"].rearrange("(k q) n -> q k n", q=128))
    brb = p.psb("brbs", [128, 20])
    p.op("sync", "dma_start", out=brb[:], in_=W["brb"])
    weins = [p.psb("weins%d" % j, [128, 8, D], BF16) for j in range(2)]
    weouts = [p.psb("weouts%d" % j, [128, 4, D], BF16) for j in range(2)]
    G = 4
    xg, acc = p.psb("xg", [128, G, D]), p.psb("acc", [128, G, D])
    h2T = p.psb("h2T", [128, G, 8, 128], BF16)
    h2Tf = p.psb("h2Tf", [128, 8, 128])
    hidT = p.psb("hidT", [128, 4, G * 128], BF16)
    sgb = [p.psb("sgb%d" % j, [128, G * 128]) for j in range(2)]
    h2b = p.psb("h2b", [128, D])
    dw = p.psb("dw", [128, G, 16])
    ss = p.psb("ss", [128, 1])
    rt = p.psb("rt", [128, 64])
    psGU, psO, psT, psR = pst[0:4], pst[4:6], [pst[6]], pst[7]
    weinv = R["wein16"].rearrange("e (k q) n -> e q k n", q=128)
    weoutv = R["weout16"].rearrange("e (k q) n -> e q k n", q=128)
    xd = R["xmid"]
    tiles_all = list(range(2, NTT)) if final else list(range(NTT))
    if not final:
        grp = [[0, 1]] + [list(range(t, t + G)) for t in range(2, NTT, G)]
    else:
        grp = [list(range(t, t + G)) for t in range(2, NTT, G)]
    hk = lambda tt: h2T.name + "%d" % tt
    for tiles in grp:
        T = len(tiles) * 128
        for tt, t in enumerate(tiles):
            r = 1 if t < 2 else 0
            xk = xg.name + "%d" % tt
            X = S(xg[:, tt, :], xk)
            p.op("sync", "dma_start", out=X, in_=xd[t * 128:(t + 1) * 128, :])
            p.op("scalar", "activation", out=h2b[:], in_=X, func=AF.Square, accum_out=ss[:])
            p.op("vector", "tensor_scalar", out=ss[:], in0=ss[:], scalar1=1.0 / D, scalar2=RMS_EPS, op0=ALU.mult, op1=ALU.add)
            p.op("scalar", "activation", out=ss[:], in_=ss[:], func=AF.Sqrt)
            p.op("vector", "reciprocal", out=ss[:], in_=ss[:])
            p.op("vector", "scalar_tensor_tensor", out=h2b[:], in0=X, scalar=ss[:, 0:1], in1=mods[r][:, D:2 * D],
                 op0=ALU.mult, op1=ALU.mult)
            p.op("vector", "tensor_tensor", out=h2b[:], in0=h2b[:], in1=mods[r][:, 0:D], op=ALU.add)
            emit_transpose(p, h2b, 8, h2Tf[:, :, :], ident, psT)
            p.op("gpsimd", "tensor_copy", out=S(h2T[:, tt, :, :].rearrange("q k t -> q (k t)"), hk(tt)),
                 in_=h2Tf[:, :, :].rearrange("q k t -> q (k t)"))
            for k in range(8):
                p.op("tensor", "matmul", out=psR[:, 0:20], lhsT=h2Tf[:, k, :], rhs=wr[:, k, :], start=(k == 0), stop=(k == 7))
            c = lambda a, b: rt[:, a:b]
            lg = c(0, 4)
            p.op("vector", "tensor_tensor", out=c(0, 20), in0=psR[:, 0:20], in1=brb[:], op=ALU.add)
            p.op("vector", "reduce_max", out=c(20, 21), in_=lg, axis=AX.X)
            p.op("vector", "tensor_scalar", out=c(24, 28), in0=lg, scalar1=c(20, 21), scalar2=None, op0=ALU.is_equal)
            p.op("vector", "tensor_scalar", out=c(21, 22), in0=c(20, 21), scalar1=-1.0, scalar2=None, op0=ALU.mult)
            p.op("scalar", "activation", out=c(28, 32), in_=lg, func=AF.Exp, bias=c(21, 22), accum_out=c(22, 23))
            p.op("vector", "reciprocal", out=c(23, 24), in_=c(22, 23))
            p.op("vector", "tensor_scalar", out=c(32, 36), in0=c(4, 8), scalar1=c(24, 25), scalar2=None, op0=ALU.mult)
            for g in range(1, 4):
                p.op("vector", "scalar_tensor_tensor", out=c(32, 36), in0=c(4 + 4 * g, 8 + 4 * g), scalar=c(24 + g, 25 + g),
                     in1=c(32, 36), op0=ALU.mult, op1=ALU.add)
            p.op("vector", "reduce_max", out=c(36, 37), in_=c(32, 36), axis=AX.X)
            p.op("vector", "tensor_scalar", out=c(40, 44), in0=c(32, 36), scalar1=c(36, 37), scalar2=None, op0=ALU.is_equal)
            p.op("vector", "scalar_tensor_tensor", out=c(44, 48), in0=c(40, 44), scalar=-1e30, in1=c(32, 36),
                 op0=ALU.mult, op1=ALU.add)
            p.op("vector", "reduce_max", out=c(37, 38), in_=c(44, 48), axis=AX.X)
            p.op("vector", "tensor_scalar", out=c(48, 52), in0=c(44, 48), scalar1=c(37, 38), scalar2=None, op0=ALU.is_equal)
            p.op("vector", "tensor_tensor", out=c(38, 39), in0=c(37, 38), in1=c(36, 37), op=ALU.subtract)
            p.op("scalar", "activation", out=c(38, 39), in_=c(38, 39), func=AF.Exp)
            p.op("vector", "tensor_scalar", out=c(39, 40), in0=c(38, 39), scalar1=1.0, scalar2=None, op0=ALU.add)
            p.op("vector", "reciprocal", out=c(39, 40), in_=c(39, 40))
            p.op("vector", "tensor_tensor", out=c(38, 39), in0=c(38, 39), in1=c(39, 40), op=ALU.mult)
            p.op("vector", "tensor_tensor", out=c(39, 40), in0=c(39, 40), in1=c(23, 24), op=ALU.mult)
            p.op("vector", "tensor_tensor", out=c(38, 39), in0=c(38, 39), in1=c(23, 24), op=ALU.mult)
            p.op("vector", "tensor_scalar", out=c(52, 56), in0=c(40, 44), scalar1=c(39, 40), scalar2=None, op0=ALU.mult)
            p.op("vector", "scalar_tensor_tensor", out=c(52, 56), in0=c(48, 52), scalar=c(38, 39), in1=c(52, 56),
                 op0=ALU.mult, op1=ALU.add)
            for g in range(4):
                p.op("vector", "tensor_scalar", out=dw[:, tt, 4 * g:4 * g + 4], in0=c(52, 56), scalar1=c(24 + g, 25 + g),
                     scalar2=None, op0=ALU.mult)
        for e in range(16):
            wein, weout = weins[e % 2], weouts[e % 2]
            for k in range(0, 8, 2):
                p.op("sync", "dma_start", out=S(wein[:, k:k + 2, :], wein.name + "%d" % k), in_=weinv[e, :, k:k + 2, :])
            p.op("sync", "dma_start", out=weout[:], in_=weoutv[e, :, :, :])
            for j in range(4):
                pg_, pu_ = psGU[(j % 2) * 2], psGU[(j % 2) * 2 + 1]
                for (pt, off) in ((pg_, 0), (pu_, 512)):
                    for k in range(8):
                        p.op("tensor", "matmul", out=pt[:, 0:T], lhsT=S(wein[:, k, off + j * 128:off + (j + 1) * 128], wein.name + "%d" % (k // 2 * 2)),
                             rhs=h2T[:, 0:len(tiles), k, :], start=(k == 0), stop=(k == 7), _r=[hk(q) for q in range(len(tiles))])
                sb_ = sgb[j % 2]
                p.op("scalar", "activation", out=sb_[:, 0:T], in_=pg_[:, 0:T], func=AF.Silu)
                p.op("vector", "tensor_tensor", out=S(hidT[:, j, 0:T], hidT.name + "%d" % j), in0=sb_[:, 0:T], in1=pu_[:, 0:T], op=ALU.mult)
            for tt, t in enumerate(tiles):
                for h in range(2):
                    po = psO[h]
                    for j in range(4):
                        p.op("tensor", "matmul", out=po[:, :], lhsT=S(hidT[:, j, tt * 128:(tt + 1) * 128], hidT.name + "%d" % j),
                             rhs=weout[:, j, h * 512:(h + 1) * 512], start=(j == 0), stop=(j == 3))
                    a = S(acc[:, tt, h * 512:(h + 1) * 512], acc.name + "%d_%d" % (tt, h))
                    if e == 0:
                        p.op("vector", "tensor_scalar", out=a, in0=po[:, :], scalar1=dw[:, tt, e:e + 1], scalar2=None, op0=ALU.mult)
                    else:
                        p.op("vector", "scalar_tensor_tensor", out=a, in0=po[:, :], scalar=dw[:, tt, e:e + 1], in1=a,
                             op0=ALU.mult, op1=ALU.add)
        for tt, t in enumerate(tiles):
            r = 1 if t < 2 else 0
            xk = xg.name + "%d" % tt
            for h in range(2):
                a = S(acc[:, tt, h * 512:(h + 1) * 512], acc.name + "%d_%d" % (tt, h))
                xh = S(xg[:, tt, h * 512:(h + 1) * 512], xk)
                p.op("vector", "tensor_tensor", out=a, in0=a, in1=mods[r][:, 2 * D + h * 512:2 * D + (h + 1) * 512], op=ALU.mult)
                p.op("vector", "tensor_tensor", out=xh, in0=xh, in1=a, op=ALU.add)
            X = S(xg[:, tt, :], xk)
            if final:
                p.op("scalar", "activation", out=h2b[:], in_=X, func=AF.Square, accum_out=ss[:])
                p.op("vector", "tensor_scalar", out=ss[:], in0=ss[:], scalar1=1.0 / D, scalar2=RMS_EPS, op0=ALU.mult, op1=ALU.add)
                p.op("scalar", "activation", out=ss[:], in_=ss[:], func=AF.Sqrt)
                p.op("vector", "reciprocal", out=ss[:], in_=ss[:])
                p.op("vector", "scalar_tensor_tensor", out=X, in0=X, scalar=ss[:, 0:1], in1=gf[:], op0=ALU.mult, op1=ALU.mult)
                p.op("sync", "dma_start", out=R["out"][(t - 2) * 128:(t - 1) * 128, :], in_=X)
            else:
                p.op("sync", "dma_start", out=R["xcur"][t * 128:(t + 1) * 128, :], in_=X)
    p.end_phase()


IN_SHAPES = dict(
    xin=[NTT * 128, D], cT=[128, 2, 8], wmod=[DEPTH, D, 6 * D], bmodb=[DEPTH, 128, 6 * D], gn1b=[DEPTH, 128, D],
    gn2b=[DEPTH, 128, D], w_in=[DEPTH, D, PROJ_W], convw=[DEPTH, 4, 128, 4, 5], convb=[DEPTH, 4, 128, 4],
    dtb=[DEPTH, 4, 128, 8], alog=[DEPTH, 4, 128, 8], dskip=[DEPTH, 4, 128, 4], poolw=[DEPTH, 4, 128, 128],
    pscale=[DEPTH, 4, 128, 128], cst=[8, 128, 128], pm2=[4, 9, 128, 128], pm1=[4, 3, 128, 128], rc=[4, 128, NCH],
    ssdg=[DEPTH, 128, D], lng=[DEPTH, 128, 512], lnb=[DEPTH, 128, 512], wsT=[DEPTH, 4, 128, 128], bsT=[DEPTH, 128, 4],
    wbs=[DEPTH, D, D], wbp=[DEPTH, 512, D], wbg=[DEPTH, 512, D], wo=[DEPTH, D, D], wr=[DEPTH, D, 20], brb=[DEPTH, 128, 20],
    wein=[DEPTH, 16, D, D], weout=[DEPTH, 16, 512, D], gfin=[128, D], ident=[128, 128])


def phase_wcast(p, R):
    p.begin_phase("wcast")
    W = R["W"]
    st = [p.psb("st%d" % j, [128, 4, D]) for j in range(3)]
    cv = [p.psb("cv%d" % j, [128, 4, D], BF16) for j in range(3)]
    srcs = []
    for e in range(16):
        vi = W["wein"][e].rearrange("(k q) n -> q k n", q=128)
        vo16 = R["wein16"][e].rearrange("(k q) n -> q k n", q=128)
        srcs += [(vi[:, 0:4, :], vo16[:, 0:4, :]), (vi[:, 4:8, :], vo16[:, 4:8, :])]
        srcs.append((W["weout"][e].rearrange("(k q) n -> q k n", q=128), R["weout16"][e].rearrange("(k q) n -> q k n", q=128)))
    for n, (src, dst) in enumerate(srcs):
        j = n % 3
        p.op("sync", "dma_start", out=st[j][:], in_=src)
        if j == 0:
            p.op("scalar", "activation", out=cv[j][:], in_=st[j][:], func=AF.Copy)
        elif j == 1:
            p.op("vector", "tensor_copy", out=cv[j][:], in_=st[j][:])
        else:
            p.op("gpsimd", "tensor_copy", out=cv[j][:], in_=st[j][:])
        p.op("sync", "dma_start", out=dst, in_=cv[j][:])
    p.end_phase()


LAYERED = ("wmod", "bmodb", "gn1b", "gn2b", "w_in", "convw", "convb", "dtb", "alog", "dskip", "poolw", "pscale", "ssdg", "lng",
           "lnb", "wsT", "bsT", "wbs", "wbp", "wbg", "wo", "wr", "brb", "wein", "weout")


def phase_final(p, R):
    p.begin_phase("fin")
    gf = p.psb("gf", [128, D])
    p.op("sync", "dma_start", out=gf[:], in_=R["W"]["gfin"])
    xt = [p.psb("xt%d" % j, [128, D]) for j in range(3)]
    jk = [p.psb("jk%d" % j, [128, D]) for j in range(2)]
    ss = [p.psb("ss%d" % j, [128, 1]) for j in range(3)]
    for t in range(2, NTT):
        b = t % 3
        p.op("sync", "dma_start", out=xt[b][:], in_=R["xcur"][t * 128:(t + 1) * 128, :])
        emit_rstd(p, xt[b][:], ss[b][:], jk[t % 2][:], RMS_EPS, D)
        p.op("vector", "scalar_tensor_tensor", out=xt[b][:], in0=xt[b][:], scalar=ss[b][:, 0:1], in1=gf[:], op0=ALU.mult, op1=ALU.mult)
        p.op("gpsimd", "dma_start", out=R["out"][(t - 2) * 128:(t - 1) * 128, :], in_=xt[b][:])
    p.end_phase()


BLOB_COLS = 8192


def blob_layout():
    offs, off = {}, 0
    for k in LAYERED:
        sh = IN_SHAPES[k][1:]
        n = int(np.prod(sh))
        offs[k] = (off, n, sh)
        off += (n + 127) // 128 * 128
    rows = (off + BLOB_COLS - 1) // BLOB_COLS
    return offs, rows


def build_fused(depth=DEPTH, phases=('l1', 'l2', 'l3a', 'l3b')):
    nc = bass.Bass("TRN2", target_bir_lowering=False)
    offs, rows = blob_layout()
    W = {k: dram_in(nc, k, sh) for k, sh in IN_SHAPES.items() if k not in LAYERED}
    blob = dram_in(nc, "blob", [DEPTH, rows, BLOB_COLS])
    out = dram_out(nc, "out", [SEQ, D])
    scr = lambda n, sh: nc.dram_tensor(n, list(sh), F32, kind="Internal").ap()
    wcur = scr("wcur", [rows, BLOB_COLS])
    flat = wcur.rearrange("r c -> (r c)")
    for k in LAYERED:
        off, n, sh = offs[k]
        names = "abcde"[:len(sh)]
        W[k] = flat[off:off + n].rearrange("(%s) -> %s" % (" ".join(names), " ".join(names)),
                                           **{nm: int(v) for nm, v in zip(names, sh)})
    R = dict(W=W, cT=W["cT"], out=out,
             xcur=scr("xcur", [NTT * 128, D]), xmid=scr("xmid", [NTT * 128, D]),
             pz=scr("pz", [NTT * 128, D]), pm=scr("pm", [NTT * 128, 1568]), pg=scr("pg", [NTT * 128, 3 * D]),
             xbc_c=scr("xbc_c", [2048, CTX + 4]), xbc_l=scr("xbc_l", [2048, SEQ + 4]),
             yf=scr("yf", [NTT * 128, D]), yb=scr("yb", [NTT * 128, D]), ypool=scr("ypool", [NTT * 128, 512]),
             wein16=nc.dram_tensor("wein16", [16, D, D], BF16, kind="Internal").ap(),
             weout16=nc.dram_tensor("weout16", [16, 512, D], BF16, kind="Internal").ap())
    p = Prog(nc)
    R["pst"] = [p.ps("ps%d" % j) for j in range(8)]
    R["ident"] = p.sb("identsb", [128, 128])
    R["cst"] = p.sb("csts", [128, 8, 128])
    p.begin_phase("init")
    p.op("sync", "dma_start", out=R["ident"][:], in_=W["ident"])
    p.op("sync", "dma_start", out=R["cst"][:], in_=W["cst"].rearrange("c q n -> q c n"))
    zt = p.psb("zeros", [128, 16, 2])
    p.op("vector", "memset", ap=zt[:], constant=0.0)
    for dst, n in ((R["xbc_c"], CTX), (R["xbc_l"], SEQ)):
        v = dst.rearrange("(j q) t -> q j t", q=128)
        p.op("sync", "dma_start", out=v[:, :, 0:2], in_=zt[:])
        p.op("sync", "dma_start", out=v[:, :, n + 2:n + 4], in_=zt[:])
    for j in range(10):
        p.op("sync", "dma_start", out=R["xcur"][j * 1664:(j + 1) * 1664, :], in_=W["xin"][j * 1664:(j + 1) * 1664, :])
    p.end_phase()
    init = p.end_segment()
    p.begin_phase("wload")
    lb = LAP(blob)
    NCP = 8
    step = (rows + NCP - 1) // NCP
    for j in range(NCP):
        r0, r1 = j * step, min(rows, (j + 1) * step)
        p.op("sync", "dma_start", out=wcur[r0:r1, :], in_=lb[r0:r1, :])
    p.end_phase()
    if 'l1' in phases:
        phase_l1(p, R)
    if 'l2' in phases:
        for g in range(4):
            phase_l2(p, R, g)
    if 'l3a' in phases:
        phase_l3a(p, R)
    if 'l3b' in phases or 'wcast' in phases:
        phase_wcast(p, R)
    if 'l3b' in phases:
        phase_l3b(p, R)
    body = p.end_segment()
    phase_final(p, R)
    epi = p.end_segment()
    p.emit_program(init, body, epi, depth)
    return nc


def rep(v, n=128):
    v = np.asarray(v, np.float32).reshape(-1)
    return np.ascontiguousarray(np.broadcast_to(v[None, :], (n, v.shape[0])))


def scan_consts():
    k = np.arange(128)[:, None]
    i = np.arange(128)[None, :]
    c = np.zeros((8, 128, 128), np.float32)
    c[0] = np.eye(128)
    c[1] = 1.0
    c[2] = (k <= i)
    c[3] = (k >= i)
    c[4] = (k > i)
    c[5] = (k < i)
    c[6] = (i >= k)
    c[7] = (i <= k)
    return c


def pool_consts(w):
    lo, hi = w // 2, w - w // 2
    pm2 = np.zeros((9, 128, 128), np.float32)
    k = np.arange(128)
    i = np.arange(128)
    for dl in range(-4, 5):
        rk = 2 * dl + k // 64
        ck = k % 64
        ri = i // 64
        ci = i % 64
        pm2[dl + 4] = ((rk[:, None] >= ri[None, :] - lo) & (rk[:, None] < ri[None, :] + hi) &
                       (ck[:, None] >= ci[None, :] - lo) & (ck[:, None] < ci[None, :] + hi))
    pm1 = np.zeros((3, 128, 128), np.float32)
    for dl in range(-1, 2):
        pk = dl * 128 + k
        pm1[dl + 1] = (pk[:, None] >= i[None, :] - lo) & (pk[:, None] < i[None, :] + hi)

    def cnt(n):
        pos = np.arange(n)
        return (np.clip(pos + hi, 0, n) - np.clip(pos - lo, 0, n)).astype(np.float32)
    c64, c256 = cnt(64), cnt(256)
    rows = np.arange(SEQ) // 64
    cols = np.arange(SEQ) % 64
    rc = np.concatenate([1.0 / cnt(CTX), 1.0 / (c256[rows] * c64[cols])]).astype(np.float32)
    return pm2, pm1, np.ascontiguousarray(rc.reshape(NCH, 128).T)


def host_inputs(W):
    cc = np.ascontiguousarray
    L = DEPTH
    m = {}
    m["wmod"] = W["w_mod"]
    m["bmodb"] = cc(np.stack([rep(W["b_mod"][i]) for i in range(L)]))
    m["gn1b"] = cc(np.stack([rep(W["g_norm1"][i]) for i in range(L)]))
    m["gn2b"] = cc(np.stack([rep(W["g_norm2"][i]) for i in range(L)]))
    m["w_in"] = W["w_in"]
    convw = np.zeros((L, 4, 128, 4, 5), np.float32)
    convb = np.zeros((L, 4, 128, 4), np.float32)
    dtb = np.zeros((L, 4, 128, 8), np.float32)
    alog = np.zeros((L, 4, 128, 8), np.float32)
    dskip = np.zeros((L, 4, 128, 4), np.float32)
    pscale = np.zeros((L, 4, 128, 128), np.float32)
    for i in range(L):
        for g in range(4):
            ch = np.r_[g * 256:(g + 1) * 256, 1024 + g * 128:1024 + (g + 1) * 128, 1536 + g * 128:1536 + (g + 1) * 128]
            convw[i, g] = W["conv_w"][i][:, ch].T.reshape(4, 128, 5).transpose(1, 0, 2)
            convb[i, g] = W["conv_b"][i][ch].reshape(4, 128).T
            hs = slice(4 * g, 4 * g + 4)
            dtb[i, g] = rep(np.concatenate([W["dt_bias"][i][0, hs], W["dt_bias"][i][1, hs]]))
            alog[i, g] = rep(np.concatenate([W["a_log"][i][0, hs], W["a_log"][i][1, hs]]))
            dskip[i, g] = rep(W["d_skip"][i][hs])
            pscale[i, g] = rep(W["pool_scale"][i][g * 128:(g + 1) * 128])
    m.update(convw=convw, convb=convb, dtb=dtb, alog=alog, dskip=dskip, pscale=pscale, poolw=W["pool_w"])
    pcs = [pool_consts(w) for w in POOL_WINDOWS]
    m["cst"] = scan_consts()
    m["pm2"] = cc(np.stack([c[0] for c in pcs]))
    m["pm1"] = cc(np.stack([c[1] for c in pcs]))
    m["rc"] = cc(np.stack([c[2] for c in pcs]))
    m["ssdg"] = cc(np.stack([rep(W["ssd_norm_g"][i]) for i in range(L)]))
    m["lng"] = cc(np.stack([rep(W["gmlp_ln_g"][i]) for i in range(L)]))
    m["lnb"] = cc(np.stack([rep(W["gmlp_ln_b"][i]) for i in range(L)]))
    m["wsT"] = cc(W["gmlp_ws"].transpose(0, 1, 3, 2))
    m["bsT"] = cc(W["gmlp_bs"].transpose(0, 2, 1))
    m.update(wbs=W["w_br_ssd"], wbp=W["w_br_pool"], wbg=W["w_br_gmlp"], wo=W["w_o"])
    m["wr"] = cc(np.concatenate([W["w_rg"], W["w_re"]], 2))
    m["brb"] = cc(np.stack([rep(np.concatenate([W["b_rg"][i], W["b_re"][i]])) for i in range(L)]))
    m.update(wein=W["w_e_in"], weout=W["w_e_out"], gfin=rep(W["g_final"]), ident=np.eye(128, dtype=np.float32))
    offs, rows = blob_layout()
    blob = np.zeros((L, rows * BLOB_COLS), np.float32)
    for k in LAYERED:
        off, n, sh = offs[k]
        a = np.asarray(m.pop(k), np.float32)
        assert list(a.shape[1:]) == list(sh), (k, a.shape, sh)
        blob[:, off:off + n] = a.reshape(L, n)
    m["blob"] = blob.reshape(L, rows, BLOB_COLS)
    return m


_NC = {}


def kernel(**inp):
    W = {k: np.asarray(v, np.float32) for k, v in inp.items()}
    shared = host_inputs(W)
    maps = []
    for k in range(NCORE):
        b = k // max(1, NCORE // 2)
        cv = np.stack([W["c"][b], W["c_ctx"]], 0)
        m = dict(shared)
        m["xin"] = np.ascontiguousarray(np.concatenate([W["ctx"][b], W["x"][b]], 0))
        m["cT"] = np.ascontiguousarray(cv.reshape(2, 8, 128).transpose(2, 0, 1))
        maps.append(m)
    if "nc" not in _NC:
        _NC["nc"] = build_fused()
    res = run_bass_kernel_spmd(_NC["nc"], maps, core_ids=list(range(NCORE)))
    return np.stack([res.results[0]["out"], res.results[NCORE // 2]["out"]], 0)
```

```python
import contextlib
import numpy as np
import concourse.bass as bass
import concourse.mybir as mybir
from concourse.bass_utils import run_bass_kernel_spmd

F32 = mybir.dt.float32
BF16 = mybir.dt.bfloat16
AF = mybir.ActivationFunctionType
ALU = mybir.AluOpType
AX = mybir.AxisListType

D = 1024
DEPTH = 4
SEQ = 16384
CTX = 256
NCORE = 8
PROJ_W = 7712
OFF_XBC, OFF_DT, OFF_POOL, OFF_GMLP, OFF_GATE = 1024, 3072, 3104, 3616, 4640
POOL_WINDOWS = (2, 4, 8, 16)
RMS_EPS, SSD_EPS, LN_EPS = 1e-6, 1e-5, 1e-5
NTT = 130
NCH = NTT
SEM_R = 14000


class S:
    def __init__(self, ap, key):
        self.ap, self.key = ap, key


class LAP:
    def __init__(self, base, tf=()):
        self.base, self.tf = base, tuple(tf)

    def __getitem__(self, idx):
        return LAP(self.base, self.tf + (("g", idx),))

    def rearrange(self, pat, **kw):
        return LAP(self.base, self.tf + (("r", pat, kw),))

    def resolve(self, i):
        nd = len(self.base.shape)
        ap = self.base[(bass.ds(i, 1),) + (slice(None),) * (nd - 1)]
        n = "abcdefgh"[:nd]
        ap = ap.rearrange("%s -> (%s %s) %s" % (" ".join(n), n[0], n[1], " ".join(n[2:])))
        for t in self.tf:
            ap = ap[t[1]] if t[0] == "g" else ap.rearrange(t[1], **t[2])
        return ap


READ_KW = ("in_", "in0", "in1", "lhsT", "rhs", "scalar", "scalar1", "scalar2", "bias", "scale",
           "identity", "data0", "data1", "initial")
WRITE_KW = ("out", "accum_out", "ap")
ENGS = ("tensor", "vector", "scalar", "gpsimd", "sync")


class Prog:
    def __init__(self, nc):
        self.nc = nc
        self.ops = {e: [] for e in ENGS}
        self.cnt = {e: 0 for e in ENGS}
        self.dcnt = {e: 0 for e in ENGS}
        self.lastw = {}
        self.readers = {}
        self.seen = {e: {} for e in ENGS}
        self.K = 12
        self.semkeys = []
        self.stack = contextlib.ExitStack()
        self.npsum = 0
        self.sems = {}
        self.tag = ''

    def sb(self, name, shape, dt=F32):
        return self.stack.enter_context(self.nc.sbuf_tensor(name, list(shape), dt))

    def ps(self, name, shape=(128, 512), dt=F32):
        return self.stack.enter_context(self.nc.psum_tensor(name, list(shape), dt))

    def _need(self, eng, tok, waits):
        if tok is None:
            return
        key, val = tok
        if eng == "tensor" and key[0] == "c" and key[1] == "tensor":
            return
        if self.seen[eng].get(key, 0) >= val:
            return
        self.seen[eng][key] = val
        waits.append(tok)

    def _regions(self, kw):
        reads, writes = [], []
        clean = {}
        for k, v in kw.items():
            key = None
            if isinstance(v, S):
                key, v = v.key, v.ap
            if isinstance(v, bass.AP):
                if key is None:
                    key = v.tensor.name
                if k in WRITE_KW:
                    writes.append(key)
                elif k in READ_KW:
                    reads.append(key)
            clean[k] = v
        return clean, reads, writes

    def op(self, eng, name, **kw):
        extra_r = kw.pop("_r", ())
        extra_w = kw.pop("_w", ())
        kw, reads, writes = self._regions(kw)
        reads = list(reads) + list(extra_r)
        writes = list(writes) + list(extra_w)
        is_dma = name == "dma_start"
        if is_dma:
            eng = "sync"
        waits = []
        for r in reads:
            self._need(eng, self.lastw.get(r), waits)
        for r in writes:
            self._need(eng, self.lastw.get(r), waits)
            for k2, v2 in self.readers.get(r, {}).items():
                self._need(eng, (k2, v2), waits)
        if is_dma:
            i = self.dcnt[eng]
            self.dcnt[eng] += 1
            key = ("d", eng, i % self.K)
            val = 16 * (i // self.K + 1)
            if i >= self.K:
                self._need(eng, (key, val - 16), waits)
            inc = 16
        else:
            self.cnt[eng] += 1
            n = self.cnt[eng] - 1
            key = ("c", eng, n // SEM_R)
            val = n % SEM_R + 1
            inc = 1
        tok = (key, val)
        if key not in self.semkeys:
            self.semkeys.append(key)
        for r in writes:
            self.lastw[r] = tok
            self.readers[r] = {}
        for r in reads:
            d = self.readers.setdefault(r, {})
            if d.get(key, 0) < val:
                d[key] = val
        self.ops[eng].append((name, kw, waits, key, inc))
        return tok

    def barrier(self):
        toks = []
        for e in ENGS:
            n = self.dcnt[e]
            for i in range(max(0, n - self.K), n):
                toks.append((("d", e, i % self.K), 16 * (i // self.K + 1)))
            if self.cnt[e]:
                n = self.cnt[e] - 1
                toks.append((("c", e, n // SEM_R), n % SEM_R + 1))
        for e in ENGS:
            waits = []
            for t in toks:
                self._need(e, t, waits)
            self.ops[e].append(("__wait__", {}, waits, None, 0))

    def begin_phase(self, tag):
        self.tag = tag
        self.pstack = contextlib.ExitStack()

    def psb(self, name, shape, dt=F32):
        return self.pstack.enter_context(self.nc.sbuf_tensor(self.tag + "_" + name, list(shape), dt))

    def end_phase(self):
        self.barrier()
        self.pstack.close()

    def end_segment(self):
        seg = self.ops
        self.ops = {e: [] for e in ENGS}
        self.cnt = {e: 0 for e in ENGS}
        self.dcnt = {e: 0 for e in ENGS}
        self.lastw, self.readers = {}, {}
        self.seen = {e: {} for e in ENGS}
        return seg

    def emit_program(self, init, body, epi, depth):
        nc = self.nc
        st = self.stack
        sems = {key: st.enter_context(nc.semaphore("s%d" % n)) for n, key in enumerate(self.semkeys)}
        SD, SG, SD2 = (st.enter_context(nc.semaphore(n)) for n in ("hs_done", "hs_go", "hs_ack"))
        MASTER = "sync"

        def run(e, lst, i):
            for name, kw, waits, key, inc in lst:
                for (k2, v2) in waits:
                    e.wait_ge(sems[k2], v2)
                if name == "__wait__":
                    continue
                kw = {k: (v.resolve(i) if isinstance(v, LAP) else v) for k, v in kw.items()}
                getattr(e, name)(**kw).then_inc(sems[key], inc)

        def handshake(e, eng):
            e.sem_inc(SD, 1)
            if eng == MASTER:
                e.wait_ge(SD, len(ENGS))
                for sm in sems.values():
                    e.sem_clear(sm)
                e.sem_clear(SD)
                e.sem_inc(SG, 1)
                e.wait_ge(SD2, len(ENGS) - 1)
                e.sem_clear(SG)
                e.sem_clear(SD2)
            else:
                e.wait_ge(SG, 1)
                e.sem_inc(SD2, 1)

        def stream(e, eng):
            run(e, init[eng], None)
            handshake(e, eng)
            with e.Fori(0, depth) as i:
                run(e, body[eng], i)
                handshake(e, eng)
            run(e, epi[eng], None)

        with nc.Block() as block:
            @block.sync
            def _(e):
                stream(e, "sync")

            @block.tensor
            def _(e):
                stream(e, "tensor")

            @block.vector
            def _(e):
                stream(e, "vector")

            @block.scalar
            def _(e):
                stream(e, "scalar")

            @block.gpsimd
            def _(e):
                stream(e, "gpsimd")
        self.stack.close()


def dram_in(nc, name, shape):
    return nc.dram_tensor(name, list(shape), F32, kind="ExternalInput").ap()


def dram_out(nc, name, shape):
    return nc.dram_tensor(name, list(shape), F32, kind="ExternalOutput").ap()


def emit_mod(p, cT, wmod, bmodb, ncol, ps_tiles, name, CWM=256):
    sc = p.psb(name + "_sc", [128, 2, 8])
    p.op("sync", "dma_start", out=sc[:], in_=cT)
    p.op("scalar", "activation", out=sc[:], in_=sc[:], func=AF.Silu)
    lh = p.psb(name + "_lh", [128, 16, 128])
    for r in range(2):
        for k in range(8):
            p.op("vector", "tensor_copy", out=S(lh[:, r * 8 + k, :], name + "_lh%d" % (r * 8 + k)),
                 in_=sc[:, r, k:k + 1].broadcast_to([128, 128]))
    bm = p.psb(name + "_bm", [128, ncol])
    p.op("sync", "dma_start", out=bm[:], in_=bmodb)
    outs = [p.psb(name + "_m%d" % r, [128, ncol]) for r in range(2)]
    wv = wmod.rearrange("(k q) n -> q k n", q=128)
    wbuf = [p.psb(name + "_w%d" % i, [128, 8, CWM]) for i in range(2)]
    for ci in range(ncol // CWM):
        wb = wbuf[ci % 2]
        p.op("sync", "dma_start", out=wb[:], in_=wv[:, :, ci * CWM:(ci + 1) * CWM])
        for r in range(2):
            pt = ps_tiles[(ci * 2 + r) % len(ps_tiles)]
            for k in range(8):
                p.op("tensor", "matmul", out=pt[:, 0:CWM], lhsT=S(lh[:, r * 8 + k, :], name + "_lh%d" % (r * 8 + k)),
                     rhs=wb[:, k, :], start=(k == 0), stop=(k == 7))
            p.op("vector", "tensor_tensor", out=outs[r][:, ci * CWM:(ci + 1) * CWM],
                 in0=pt[:, 0:CWM], in1=bm[:, ci * CWM:(ci + 1) * CWM], op=ALU.add)
    return outs


def emit_rstd(p, src, ss, junk, eps, n):
    p.op("scalar", "activation", out=junk, in_=src, func=AF.Square, accum_out=ss)
    p.op("vector", "tensor_scalar", out=ss, in0=ss, scalar1=1.0 / n, scalar2=eps, op0=ALU.mult, op1=ALU.add)
    p.op("scalar", "activation", out=ss, in_=ss, func=AF.Sqrt)
    p.op("vector", "reciprocal", out=ss, in_=ss)


def emit_transpose(p, src, nblk, dstT, ident, ps_tiles, eng_cycle=("scalar", "vector"), key=None, skey=None, dst2=None, key2=None):
    for q in range((nblk + 3) // 4):
        pt = ps_tiles[q % len(ps_tiles)]
        nb = min(4, nblk - q * 4)
        for j in range(nb):
            jj = q * 4 + j
            si = src[:, jj * 128:(jj + 1) * 128]
            p.op("tensor", "transpose", out=pt[:, j * 128:(j + 1) * 128], in_=(S(si, skey) if skey else si),
                 identity=ident[:, :])
        e = eng_cycle[q % len(eng_cycle)]
        o = dstT[:, q * 4:q * 4 + nb, :]
        if key:
            o = S(o, key)
        i = pt[:, 0:nb * 128].rearrange("q (j t) -> q j t", j=nb)
        if e == "scalar":
            p.op("scalar", "activation", out=o, in_=i, func=AF.Copy)
        else:
            p.op("vector", "tensor_copy", out=o, in_=i)
        if dst2 is not None:
            o2 = dst2[:, q * 4:q * 4 + nb, :]
            if key2:
                o2 = S(o2, key2)
            if e == "scalar":
                p.op("vector", "tensor_copy", out=o2, in_=i)
            else:
                p.op("scalar", "activation", out=o2, in_=i, func=AF.Copy)


def phase_l1(p, R):
    p.begin_phase("l1")
    pst, ident = R["pst"], R["ident"]
    W = R["W"]
    mods = emit_mod(p, R["cT"], W["wmod"][:, 0:2048], W["bmodb"][:, 0:2048], 2048, pst[0:4], "mod")
    gn = p.psb("gn", [128, D])
    p.op("sync", "dma_start", out=gn[:], in_=W["gn1b"])
    gmul = [p.psb("gmul%d" % r, [128, D]) for r in range(2)]
    for r in range(2):
        p.op("vector", "scalar_tensor_tensor", out=gmul[r][:], in0=mods[r][:, D:2 * D], scalar=1.0, in1=gn[:],
             op0=ALU.add, op1=ALU.mult)
    HALF = 16
    hT = p.psb("hT", [128, HALF, 8, 128])
    xt = [p.psb("xt%d" % j, [128, D]) for j in range(2)]
    hh = [p.psb("hh%d" % j, [128, D]) for j in range(2)]
    ss = [p.psb("ss%d" % j, [128, 1]) for j in range(2)]
    wv = W["w_in"].rearrange("(k q) n -> q k n", q=128)
    wb = [p.psb("wb%d" % j, [128, 8, 512]) for j in range(2)]
    ob = [p.psb("ob%d" % j, [128, 512]) for j in range(4)]
    blocks = ([(R["pz"], 0, j * 512, 512) for j in range(2)] + [(R["pm"], 3072, 3072 + j * 392, 392) for j in range(4)] +
              [(R["pg"], OFF_GATE, OFF_GATE + j * 512, 512) for j in range(6)])
    xin = R["xcur"]
    it = 0
    wi = 0
    tiles_all = list(range(NTT))
    groups = [tiles_all[0:2] + tiles_all[2:2 + HALF - 2]]
    rest = tiles_all[HALF:]
    groups += [rest[j:j + HALF] for j in range(0, len(rest), HALF)]
    for tiles in groups:
        t0 = tiles[0]
        for t in tiles:
            r = 1 if t < 2 else 0
            b = t % 2
            p.op("sync", "dma_start", out=xt[b][:], in_=xin[t * 128:(t + 1) * 128, :])
            emit_rstd(p, xt[b][:], ss[b][:], hh[b][:], RMS_EPS, D)
            p.op("vector", "scalar_tensor_tensor", out=hh[b][:], in0=xt[b][:], scalar=ss[b][:, 0:1], in1=gmul[r][:],
                 op0=ALU.mult, op1=ALU.mult)
            p.op("vector", "tensor_tensor", out=hh[b][:], in0=hh[b][:], in1=mods[r][:, 0:D], op=ALU.add)
            emit_transpose(p, hh[b], 8, hT[:, t - t0, :, :], ident, pst[0:4], key=hT.name + "%d" % (t - t0))
        for (pdst, pbase, c0, cw) in blocks:
            w = wb[wi % 2]
            wi += 1
            p.op("sync", "dma_start", out=w[:, :, 0:cw], in_=wv[:, :, c0:c0 + cw])
            for t in tiles:
                pt = pst[4 + it % 4]
                o = ob[it % 4]
                for k in range(8):
                    p.op("tensor", "matmul", out=pt[:, 0:cw], lhsT=S(hT[:, t - t0, k, :], hT.name + "%d" % (t - t0)),
                         rhs=w[:, k, 0:cw], start=(k == 0), stop=(k == 7))
                if it % 2 == 0:
                    p.op("scalar", "activation", out=o[:, 0:cw], in_=pt[:, 0:cw], func=AF.Copy)
                else:
                    p.op("vector", "tensor_copy", out=o[:, 0:cw], in_=pt[:, 0:cw])
                p.op("sync", "dma_start", out=pdst[t * 128:(t + 1) * 128, c0 - pbase:c0 - pbase + cw], in_=o[:, 0:cw])
                it += 1
        subs = []
        lat = [t for t in tiles if t >= 2]
        if tiles[0] < 2:
            subs.append((R["xbc_c"], 0, [0, 1]))
        for j in range(0, len(lat), 4):
            sub = lat[j:j + 4]
            subs.append((R["xbc_l"], (sub[0] - 2) * 128, sub))
        for cb in range(4):
            w = wb[wi % 2]
            wi += 1
            p.op("sync", "dma_start", out=w[:], in_=wv[:, :, 1024 + cb * 512:1024 + (cb + 1) * 512])
            for cc in range(4):
                for (dst, tok0, sub) in subs:
                    n = len(sub) * 128
                    a = sub[0] - t0
                    pt = pst[4 + it % 4]
                    o = ob[it % 4]
                    for k in range(8):
                        p.op("tensor", "matmul", out=pt[:, 0:n], lhsT=w[:, k, cc * 128:(cc + 1) * 128],
                             rhs=hT[:, a:a + len(sub), k, :], start=(k == 0), stop=(k == 7),
                             _r=[hT.name + "%d" % (a + q) for q in range(len(sub))])
                    if it % 2 == 0:
                        p.op("scalar", "activation", out=o[:, 0:n], in_=pt[:, 0:n], func=AF.Copy)
                    else:
                        p.op("vector", "tensor_copy", out=o[:, 0:n], in_=pt[:, 0:n])
                    row0 = (cb * 4 + cc) * 128
                    p.op("sync", "dma_start", out=dst[row0:row0 + 128, 2 + tok0:2 + tok0 + n], in_=o[:, 0:n])
                    it += 1
    p.end_phase()


def phase_l2(p, R, g):
    p.begin_phase("l2_%d" % g)
    W = R["W"]
    pst = R["pst"]
    cst, IDENT = R["cst"], R["ident"][:, :]
    ONES = cst[:, 1, :]
    TRI = [cst[:, 2, :], cst[:, 3, :]]
    SL = [cst[:, 4, :], cst[:, 5, :]]
    MASK = [cst[:, 6, :], cst[:, 7, :]]
    youts = [R["yf"], R["yb"]]
    ypool = R["ypool"]
    pm2 = p.psb("pm2s", [128, 9, 128])
    p.op("sync", "dma_start", out=pm2[:], in_=W["pm2"][g].rearrange("c q n -> q c n"))
    pm1 = p.psb("pm1s", [128, 3, 128])
    p.op("sync", "dma_start", out=pm1[:], in_=W["pm1"][g].rearrange("c q n -> q c n"))
    small = {}
    for nm, dd, sh in (("convw", W["convw"][g], [128, 4, 5]), ("convb", W["convb"][g], [128, 4]),
                       ("dtb", W["dtb"][g], [128, 8]), ("alog", W["alog"][g], [128, 8]),
                       ("dskip", W["dskip"][g], [128, 4]), ("poolw", W["poolw"][g], [128, 128]),
                       ("pscale", W["pscale"][g], [128, 128]), ("rc", W["rc"][g], [128, NCH])):
        small[nm] = p.psb(nm + "s", sh)
        p.op("sync", "dma_start", out=small[nm][:], in_=dd)
    convw, convb, dskip, poolw, pscale, rc = (small[k] for k in ("convw", "convb", "dskip", "poolw", "pscale", "rc"))
    pp = p.psb("ppall", [128, NCH, 128])
    pv = R["pm"].rearrange("(t q) c -> q t c", q=128)
    for t0 in range(0, NCH, 13):
        p.op("gpsimd", "dma_start", out=S(pp[:, t0:t0 + 13, :], pp.name + "%d" % (t0 // 13)),
             in_=pv[:, t0:t0 + 13, 32 + g * 128:32 + (g + 1) * 128])
    dta = p.psb("dta", [128, NCH, 8])
    dA = p.psb("dA", [128, NCH, 8])
    tmpa = p.psb("tmpa", [128, NCH, 8])
    for t0 in range(0, NCH, 13):
        for d in range(2):
            c0 = 16 * d + 4 * g
            p.op("gpsimd", "dma_start", out=dta[:, t0:t0 + 13, 4 * d:4 * d + 4], in_=pv[:, t0:t0 + 13, c0:c0 + 4])
    bc8 = lambda a: a[:, :].unsqueeze(1).broadcast_to([128, NCH, 8])
    p.op("vector", "tensor_tensor", out=dta[:], in0=dta[:], in1=bc8(small["dtb"]), op=ALU.add)
    p.op("scalar", "activation", out=tmpa[:], in_=dta[:], func=AF.Abs)
    p.op("scalar", "activation", out=tmpa[:], in_=tmpa[:], func=AF.Exp, scale=-1.0)
    p.op("scalar", "activation", out=tmpa[:], in_=tmpa[:], func=AF.Ln, bias=1.0)
    p.op("vector", "tensor_scalar", out=dta[:], in0=dta[:], scalar1=0.0, scalar2=None, op0=ALU.max)
    p.op("vector", "tensor_tensor", out=dta[:], in0=dta[:], in1=tmpa[:], op=ALU.add)
    aneg = p.psb("aneg", [128, 8])
    p.op("scalar", "activation", out=aneg[:], in_=small["alog"][:], func=AF.Exp)
    p.op("vector", "tensor_scalar", out=aneg[:], in0=aneg[:], scalar1=-1.0, scalar2=None, op0=ALU.mult)
    p.op("vector", "tensor_tensor", out=dA[:], in0=dta[:], in1=bc8(aneg), op=ALU.mult)
    hst = [p.psb("hst%d" % d, [128, 256]) for d in range(2)]
    for d in range(2):
        p.op("vector", "memset", ap=hst[d][:], constant=0.0)
    NS = 2
    mk = lambda nm, sh: [p.psb("%s%d" % (nm, j), sh) for j in range(NS)]
    xr, xc, xtmp = mk("xr", [128, 4, 132]), mk("xc", [128, 4, 128]), mk("xtmp", [128, 4, 128])
    tok, sm = mk("tok", [128, 384]), mk("sm", [128, 32])
    xdt, xdtw = mk("xdt", [128, 256]), mk("xdtw", [128, 256])
    cbm, Lh, Eb, MT = mk("cbm", [128, 128]), mk("Lh", [128, 4, 128]), mk("Eb", [128, 4, 128]), mk("MT", [128, 4, 128])
    ysb, ytmp = mk("ysb", [128, 256]), mk("ytmp", [128, 256])
    pmt, pmT, ypb = mk("pmt", [128, 128]), mk("pmT", [128, 128]), mk("ypb", [128, 128])
    psT, psS, psC, psE, psY, psO, psH, psP = pst
    h4 = lambda a: a.rearrange("q (h e) -> q h e", h=4)
    b64 = lambda a: a.unsqueeze(2).broadcast_to([128, 4, 64])
    wbc = lambda k: convw[:, :, k:k + 1].broadcast_to([128, 4, 128])

    def pool_tile(ti, s):
        if ti < 2:
            PM, rad, lo, hi = pm1, 1, 0, 2
        else:
            PM, rad, lo, hi = pm2, 4, 2, NCH
        ds = [dl for dl in range(-rad, rad + 1) if lo <= ti + dl < hi]
        for n, dl in enumerate(ds):
            tj = ti + dl
            p.op("tensor", "matmul", out=psP[:, 0:128], lhsT=PM[:, dl + rad, :], rhs=S(pp[:, tj, :], pp.name + "%d" % (tj // 13)),
                 start=(n == 0), stop=(n == len(ds) - 1))
        p.op("vector", "scalar_tensor_tensor", out=pmt[s][:], in0=psP[:, 0:128], scalar=rc[:, ti:ti + 1],
             in1=S(pp[:, ti, :], pp.name + "%d" % (ti // 13)), op0=ALU.mult, op1=ALU.subtract)
        p.op("tensor", "transpose", out=psP[:, 128:256], in_=pmt[s][:], identity=IDENT)
        p.op("scalar", "activation", out=pmT[s][:], in_=psP[:, 128:256], func=AF.Copy)
        p.op("tensor", "matmul", out=psP[:, 256:384], lhsT=pmT[s][:], rhs=poolw[:], start=True, stop=True)
        p.op("vector", "tensor_tensor", out=ypb[s][:], in0=psP[:, 256:384], in1=pscale[:], op=ALU.mult)
        p.op("gpsimd", "dma_start", out=ypool[ti * 128:(ti + 1) * 128, g * 128:(g + 1) * 128], in_=ypb[s][:])

    def load(ci, s):
        src, lc = (R["xbc_c"], ci) if ci < 2 else (R["xbc_l"], ci - 2)
        cs = slice(lc * 128, lc * 128 + 132)
        p.op("sync", "dma_start", out=xr[s][:, 0:2, :], in_=src[g * 256:(g + 1) * 256, :].rearrange("(j q) t -> q j t", q=128)[:, :, cs])
        p.op("sync", "dma_start", out=xr[s][:, 2, :], in_=src[1024 + g * 128:1024 + (g + 1) * 128, cs])
        p.op("sync", "dma_start", out=xr[s][:, 3, :], in_=src[1536 + g * 128:1536 + (g + 1) * 128, cs])

    def chunk(ci, d, s):
        p.op("vector", "tensor_tensor", out=xc[s][:], in0=xr[s][:, :, 0:128], in1=wbc(0), op=ALU.mult)
        for k in range(1, 5):
            p.op("gpsimd" if k % 2 else "vector", "tensor_tensor", out=xtmp[s][:], in0=xr[s][:, :, k:k + 128], in1=wbc(k), op=ALU.mult)
            p.op("vector", "tensor_tensor", out=xc[s][:], in0=xc[s][:], in1=xtmp[s][:], op=ALU.add)
        p.op("vector", "tensor_tensor", out=xc[s][:], in0=xc[s][:], in1=convb[:, :].unsqueeze(2).broadcast_to([128, 4, 128]), op=ALU.add)
        p.op("scalar", "activation", out=xc[s][:], in_=xc[s][:], func=AF.Silu)
        for j in range(3):
            p.op("tensor", "transpose", out=psT[:, j * 128:(j + 1) * 128], in_=xc[s][:, j, :], identity=IDENT)
        p.op("scalar", "activation", out=tok[s][:], in_=psT[:, 0:384], func=AF.Copy)
        dA4 = dA[:, ci, d * 4:(d + 1) * 4]
        dt4 = dta[:, ci, d * 4:(d + 1) * 4]
        p.op("tensor", "matmul", out=psS[:, 0:4], lhsT=TRI[d], rhs=dA4, start=True, stop=True)
        p.op("tensor", "matmul", out=psS[:, 8:12], lhsT=ONES, rhs=dA4, start=True, stop=True)
        m = sm[s]
        p.op("vector", "tensor_copy", out=m[:, 0:12], in_=psS[:, 0:12])
        p.op("vector", "tensor_tensor", out=m[:, 12:16], in0=m[:, 8:12], in1=m[:, 0:4], op=ALU.subtract)
        p.op("scalar", "activation", out=m[:, 12:16], in_=m[:, 12:16], func=AF.Exp)
        p.op("scalar", "activation", out=m[:, 16:20], in_=m[:, 0:4], func=AF.Exp)
        p.op("scalar", "activation", out=m[:, 20:24], in_=m[:, 8:12], func=AF.Exp)
        p.op("vector", "tensor_tensor", out=m[:, 24:28], in0=dt4, in1=m[:, 12:16], op=ALU.mult)
        xs4 = h4(tok[s][:, 0:256])
        p.op("gpsimd", "tensor_tensor", out=h4(xdt[s][:, :]), in0=xs4, in1=b64(dt4), op=ALU.mult)
        p.op("gpsimd", "tensor_tensor", out=h4(xdtw[s][:, :]), in0=xs4, in1=b64(m[:, 24:28]), op=ALU.mult)
        p.op("tensor", "matmul", out=psC[:, 0:128], lhsT=xc[s][:, 2, :], rhs=xc[s][:, 3, :], start=True, stop=True)
        p.op("vector", "tensor_tensor", out=cbm[s][:], in0=psC[:, 0:128], in1=MASK[d], op=ALU.mult)
        p.op("vector", "tensor_tensor", out=Lh[s][:], in0=SL[d].unsqueeze(1).broadcast_to([128, 4, 128]),
             in1=dA4.unsqueeze(2).broadcast_to([128, 4, 128]), op=ALU.mult)
        for h in range(4):
            p.op("tensor", "matmul", out=psE[:, h * 128:(h + 1) * 128], lhsT=Lh[s][:, h, :], rhs=TRI[d],
                 start=True, stop=True)
        p.op("scalar", "activation", out=Eb[s][:], in_=psE[:, :].rearrange("q (h e) -> q h e", h=4), func=AF.Exp)
        p.op("vector", "tensor_tensor", out=MT[s][:], in0=Eb[s][:], in1=cbm[s][:, :].unsqueeze(1).broadcast_to([128, 4, 128]),
             op=ALU.mult)
        for h in range(4):
            p.op("tensor", "matmul", out=psY[:, h * 64:(h + 1) * 64], lhsT=MT[s][:, h, :], rhs=xdt[s][:, h * 64:(h + 1) * 64],
                 start=True, stop=True)
        p.op("tensor", "matmul", out=psO[:, 0:256], lhsT=xc[s][:, 3, :], rhs=hst[d][:], start=True, stop=True)
        p.op("vector", "tensor_tensor", out=h4(ysb[s][:, :]), in0=h4(psO[:, 0:256]), in1=b64(m[:, 16:20]), op=ALU.mult)
        p.op("vector", "tensor_tensor", out=ysb[s][:], in0=ysb[s][:], in1=psY[:, 0:256], op=ALU.add)
        if d == 0:
            p.op("gpsimd", "tensor_tensor", out=h4(ytmp[s][:, :]), in0=xs4, in1=b64(dskip[:, :]), op=ALU.mult)
            p.op("vector", "tensor_tensor", out=ysb[s][:], in0=ysb[s][:], in1=ytmp[s][:], op=ALU.add)
        p.op("sync", "dma_start", out=youts[d][ci * 128:(ci + 1) * 128, g * 256:(g + 1) * 256], in_=ysb[s][:])
        p.op("tensor", "matmul", out=psH[:, 0:256], lhsT=tok[s][:, 256:384], rhs=xdtw[s][:], start=True, stop=True)
        p.op("vector", "tensor_tensor", out=h4(hst[d][:, :]), in0=h4(hst[d][:, :]), in1=b64(m[:, 20:24]), op=ALU.mult)
        p.op("vector", "tensor_tensor", out=hst[d][:], in0=hst[d][:], in1=psH[:, 0:256], op=ALU.add)

    order = [(ci, 0) for ci in range(NCH)] + [(ci, 1) for ci in [1, 0] + list(range(NCH - 1, 1, -1))]
    load(order[0][0], 0)
    for it, (ci, d) in enumerate(order):
        if it + 1 < len(order):
            load(order[it + 1][0], (it + 1) % NS)
        chunk(ci, d, it % NS)
        if d == 0:
            pool_tile(ci, it % NS)
    p.end_phase()


def phase_l3a(p, R):
    p.begin_phase("l3a")
    W = R["W"]
    pst, ident = R["pst"], R["ident"]
    psA, psB, psC, psT = pst[0:2], pst[2:4], pst[4:6], pst[6:8]
    g1 = emit_mod(p, R["cT"], W["wmod"][:, 2048:3072], W["bmodb"][:, 2048:3072], D, pst[0:4], "mod")

    def load(name, shape, view, eng="sync"):
        t = p.psb(name, shape)
        p.op(eng, "dma_start", out=t[:], in_=view)
        return t
    ssdg, lng, lnb = load("ssdgs", [128, D], W["ssdg"]), load("lngs", [128, 512], W["lng"]), load("lnbs", [128, 512], W["lnb"])
    wsT = load("wsTs", [128, 4, 128], W["wsT"].rearrange("g q n -> q g n"))
    bsT = load("bsTs", [128, 4], W["bsT"])
    kq = lambda a: a.rearrange("(k q) n -> q k n", q=128)
    wbs = load("wbss", [128, 8, D], kq(W["wbs"]))
    wbp = load("wbps", [128, 4, D], kq(W["wbp"]))
    wbg = load("wbgs", [128, 4, D], kq(W["wbg"]))
    wo = load("wos", [128, 8, D], kq(W["wo"]))
    xt, zt, yft, ybt, uvt = (p.psb(n, [128, D]) for n in ("xt", "zt", "yft", "ybt", "uvt"))
    ypt = p.psb("ypt", [128, 512])
    gtt = p.psb("gtt", [128, 3 * D])
    t1, mg = p.psb("t1", [128, D]), p.psb("mg", [128, D])
    vn, gm = p.psb("vn", [128, 512]), p.psb("gm", [128, 512])
    TT = p.psb("TT", [128, 8, 128])
    ss, st6, mv = p.psb("ss", [128, 1]), p.psb("st6", [128, 6]), p.psb("mv", [128, 2])
    H = lambda a, h: a[:, h * 512:(h + 1) * 512]
    for t in range(NTT):
        r = 1 if t < 2 else 0
        rows = slice(t * 128, (t + 1) * 128)
        srcs = ((zt, R["pz"][rows, :]), (yft, R["yf"][rows, :]), (ybt, R["yb"][rows, :]), (uvt, R["pm"][rows, 544:1568]),
                (gtt, R["pg"][rows, :]), (xt, R["xcur"][rows, :]), (ypt, R["ypool"][rows, :]))
        for j, (tt_, dd_) in enumerate(srcs):
            p.op("sync" if j % 2 == 0 else "gpsimd", "dma_start", out=tt_[:], in_=dd_)
        p.op("vector", "tensor_tensor", out=yft[:], in0=yft[:], in1=ybt[:], op=ALU.add)
        p.op("scalar", "activation", out=zt[:], in_=zt[:], func=AF.Silu)
        p.op("vector", "tensor_tensor", out=t1[:], in0=yft[:], in1=zt[:], op=ALU.mult)
        emit_rstd(p, t1[:], ss[:], zt[:], SSD_EPS, D)
        p.op("vector", "scalar_tensor_tensor", out=t1[:], in0=t1[:], scalar=ss[:, 0:1], in1=ssdg[:], op0=ALU.mult, op1=ALU.mult)
        emit_transpose(p, t1, 8, TT[:, :, :], ident, psT)
        for h in range(2):
            for k in range(8):
                p.op("tensor", "matmul", out=psA[h][:, :], lhsT=TT[:, k, :], rhs=wbs[:, k, h * 512:(h + 1) * 512],
                     start=(k == 0), stop=(k == 7))
        p.op("scalar", "activation", out=uvt[:], in_=uvt[:], func=AF.Gelu_apprx_tanh)
        p.op("vector", "bn_stats", out=st6[:], in_=uvt[:, 512:1024])
        p.op("vector", "bn_aggr", out=mv[:], in_=st6[:])
        p.op("vector", "tensor_scalar", out=mv[:, 1:2], in0=mv[:, 1:2], scalar1=LN_EPS, scalar2=None, op0=ALU.add)
        p.op("scalar", "activation", out=mv[:, 1:2], in_=mv[:, 1:2], func=AF.Sqrt)
        p.op("vector", "reciprocal", out=mv[:, 1:2], in_=mv[:, 1:2])
        p.op("vector", "tensor_scalar", out=vn[:], in0=uvt[:, 512:1024], scalar1=mv[:, 0:1], scalar2=mv[:, 1:2],
             op0=ALU.subtract, op1=ALU.mult)
        p.op("vector", "tensor_tensor", out=vn[:], in0=vn[:], in1=lng[:], op=ALU.mult)
        p.op("vector", "tensor_tensor", out=vn[:], in0=vn[:], in1=lnb[:], op=ALU.add)
        for g in range(4):
            p.op("tensor", "matmul", out=psB[0][:, g * 128:(g + 1) * 128], lhsT=wsT[:, g, :], rhs=vn[:, g * 128:(g + 1) * 128],
                 start=True, stop=True)
        for g in range(4):
            gs = slice(g * 128, (g + 1) * 128)
            p.op("vector", "scalar_tensor_tensor", out=gm[:, gs], in0=psB[0][:, gs], scalar=bsT[:, g:g + 1], in1=uvt[:, gs],
                 op0=ALU.add, op1=ALU.mult)
        emit_transpose(p, gm, 4, TT[:, 0:4, :], ident, psT)
        for h in range(2):
            for k in range(4):
                p.op("tensor", "matmul", out=psB[h][:, :], lhsT=TT[:, k, :], rhs=wbg[:, k, h * 512:(h + 1) * 512],
                     start=(k == 0), stop=(k == 3))
        emit_transpose(p, ypt, 4, TT[:, 4:8, :], ident, psT)
        for h in range(2):
            for k in range(4):
                p.op("tensor", "matmul", out=psC[h][:, :], lhsT=TT[:, 4 + k, :], rhs=wbp[:, k, h * 512:(h + 1) * 512],
                     start=(k == 0), stop=(k == 3))
        p.op("scalar", "activation", out=gtt[:], in_=gtt[:], func=AF.Sigmoid)
        for h in range(2):
            p.op("vector", "tensor_tensor", out=H(mg, h), in0=H(gtt, h), in1=psA[h][:, :], op=ALU.mult)
            p.op("vector", "tensor_tensor", out=H(t1, h), in0=H(gtt, 2 + h), in1=psC[h][:, :], op=ALU.mult)
            p.op("vector", "tensor_tensor", out=H(mg, h), in0=H(mg, h), in1=H(t1, h), op=ALU.add)
            p.op("vector", "tensor_tensor", out=H(t1, h), in0=H(gtt, 4 + h), in1=psB[h][:, :], op=ALU.mult)
            p.op("vector", "tensor_tensor", out=H(mg, h), in0=H(mg, h), in1=H(t1, h), op=ALU.add)
        emit_transpose(p, mg, 8, TT[:, :, :], ident, psT)
        for h in range(2):
            for k in range(8):
                p.op("tensor", "matmul", out=psA[h][:, :], lhsT=TT[:, k, :], rhs=wo[:, k, h * 512:(h + 1) * 512],
                     start=(k == 0), stop=(k == 7))
            p.op("vector", "tensor_tensor", out=H(t1, h), in0=psA[h][:, :], in1=H(g1[r], h), op=ALU.mult)
            p.op("vector", "tensor_tensor", out=H(xt, h), in0=H(xt, h), in1=H(t1, h), op=ALU.add)
        p.op("sync", "dma_start", out=R["xmid"][rows, :], in_=xt[:])
    p.end_phase()


def phase_l3b(p, R):
    final = False
    p.begin_phase("l3b")
    W = R["W"]
    pst, ident = R["pst"], R["ident"]
    mods = emit_mod(p, R["cT"], W["wmod"][:, 3072:6144], W["bmodb"][:, 3072:6144], 3 * D, pst[0:4], "mod")
    gn2 = p.psb("gn2s", [128, D])
    p.op("sync", "dma_start", out=gn2[:], in_=W["gn2b"])
    for r in range(2):
        p.op("vector", "scalar_tensor_tensor", out=mods[r][:, D:2 * D], in0=mods[r][:, D:2 * D], scalar=1.0, in1=gn2[:],
             op0=ALU.add, op1=ALU.mult)
    gf = gn2
    if final:
        p.op("sync", "dma_start", out=gf[:], in_=W["gfin"])
    wr = p.psb("wrs", [128, 8, 20])
    p.op("sync", "dma_start", out=wr[:], in_=W["wr"].rearrange("(k q) n -> q k n", q=128))
    brb = p.psb("brbs", [128, 20])
    p.op("sync", "dma_start", out=brb[:], in_=W["brb"])
    weins = [p.psb("weins%d" % j, [128, 8, D], BF16) for j in range(2)]
    weouts = [p.psb("weouts%d" % j, [128, 4, D], BF16) for j in range(2)]
    G = 4
    xg, acc = p.psb("xg", [128, G, D]), p.psb("acc", [128, G, D])
    h2T = p.psb("h2T", [128, G, 8, 128], BF16)
    h2Tf = p.psb("h2Tf", [128, 8, 128])
    hidT = p.psb("hidT", [128, 4, G * 128], BF16)
    sgb = [p.psb("sgb%d" % j, [128, G * 128]) for j in range(2)]
    h2b = p.psb("h2b", [128, D])
    dw = p.psb("dw", [128, G, 16])
    ss = p.psb("ss", [128, 1])
    rt = p.psb("rt", [128, 64])
    psGU, psO, psT, psR = pst[0:4], pst[4:6], [pst[6]], pst[7]
    weinv = R["wein16"].rearrange("e (k q) n -> e q k n", q=128)
    weoutv = R["weout16"].rearrange("e (k q) n -> e q k n", q=128)
    xd = R["xmid"]
    tiles_all = list(range(2, NTT)) if final else list(range(NTT))
    if not final:
        grp = [[0, 1]] + [list(range(t, t + G)) for t in range(2, NTT, G)]
    else:
        grp = [list(range(t, t + G)) for t in range(2, NTT, G)]
    hk = lambda tt: h2T.name + "%d" % tt
    for tiles in grp:
        T = len(tiles) * 128
        for tt, t in enumerate(tiles):
            r = 1 if t < 2 else 0
            xk = xg.name + "%d" % tt
            X = S(xg[:, tt, :], xk)
            p.op("sync", "dma_start", out=X, in_=xd[t * 128:(t + 1) * 128, :])
            p.op("scalar", "activation", out=h2b[:], in_=X, func=AF.Square, accum_out=ss[:])
            p.op("vector", "tensor_scalar", out=ss[:], in0=ss[:], scalar1=1.0 / D, scalar2=RMS_EPS, op0=ALU.mult, op1=ALU.add)
            p.op("scalar", "activation", out=ss[:], in_=ss[:], func=AF.Sqrt)
            p.op("vector", "reciprocal", out=ss[:], in_=ss[:])
            p.op("vector", "scalar_tensor_tensor", out=h2b[:], in0=X, scalar=ss[:, 0:1], in1=mods[r][:, D:2 * D],
                 op0=ALU.mult, op1=ALU.mult)
            p.op("vector", "tensor_tensor", out=h2b[:], in0=h2b[:], in1=mods[r][:, 0:D], op=ALU.add)
            emit_transpose(p, h2b, 8, h2Tf[:, :, :], ident, psT)
            p.op("gpsimd", "tensor_copy", out=S(h2T[:, tt, :, :].rearrange("q k t -> q (k t)"), hk(tt)),
                 in_=h2Tf[:, :, :].rearrange("q k t -> q (k t)"))
            for k in range(8):
                p.op("tensor", "matmul", out=psR[:, 0:20], lhsT=h2Tf[:, k, :], rhs=wr[:, k, :], start=(k == 0), stop=(k == 7))
            c = lambda a, b: rt[:, a:b]
            lg = c(0, 4)
            p.op("vector", "tensor_tensor", out=c(0, 20), in0=psR[:, 0:20], in1=brb[:], op=ALU.add)
            p.op("vector", "reduce_max", out=c(20, 21), in_=lg, axis=AX.X)
            p.op("vector", "tensor_scalar", out=c(24, 28), in0=lg, scalar1=c(20, 21), scalar2=None, op0=ALU.is_equal)
            p.op("vector", "tensor_scalar", out=c(21, 22), in0=c(20, 21), scalar1=-1.0, scalar2=None, op0=ALU.mult)
            p.op("scalar", "activation", out=c(28, 32), in_=lg, func=AF.Exp, bias=c(21, 22), accum_out=c(22, 23))
            p.op("vector", "reciprocal", out=c(23, 24), in_=c(22, 23))
            p.op("vector", "tensor_scalar", out=c(32, 36), in0=c(4, 8), scalar1=c(24, 25), scalar2=None, op0=ALU.mult)
            for g in range(1, 4):
                p.op("vector", "scalar_tensor_tensor", out=c(32, 36), in0=c(4 + 4 * g, 8 + 4 * g), scalar=c(24 + g, 25 + g),
                     in1=c(32, 36), op0=ALU.mult, op1=ALU.add)
            p.op("vector", "reduce_max", out=c(36, 37), in_=c(32, 36), axis=AX.X)
            p.op("vector", "tensor_scalar", out=c(40, 44), in0=c(32, 36), scalar1=c(36, 37), scalar2=None, op0=ALU.is_equal)
            p.op("vector", "scalar_tensor_tensor", out=c(44, 48), in0=c(40, 44), scalar=-1e30, in1=c(32, 36),
                 op0=ALU.mult, op1=ALU.add)
            p.op("vector", "reduce_max", out=c(37, 38), in_=c(44, 48), axis=AX.X)
            p.op("vector", "tensor_scalar", out=c(48, 52), in0=c(44, 48), scalar1=c(37, 38), scalar2=None, op0=ALU.is_equal)
            p.op("vector", "tensor_tensor", out=c(38, 39), in0=c(37, 38), in1=c(36, 37), op=ALU.subtract)
            p.op("scalar", "activation", out=c(38, 39), in_=c(38, 39), func=AF.Exp)
            p.op("vector", "tensor_scalar", out=c(39, 40), in0=c(38, 39), scalar1=1.0, scalar2=None, op0=ALU.add)
            p.op("vector", "reciprocal", out=c(39, 40), in_=c(39, 40))
            p.op("vector", "tensor_tensor", out=c(38, 39), in0=c(38, 39), in1=c(39, 40), op=ALU.mult)
            p.op("vector", "tensor_tensor", out=c(39, 40), in0=c(39, 40), in1=c(23, 24), op=ALU.mult)
            p.op("vector", "tensor_tensor", out=c(38, 39), in0=c(38, 39), in1=c(23, 24), op=ALU.mult)
            p.op("vector", "tensor_scalar", out=c(52, 56), in0=c(40, 44), scalar1=c(39, 40), scalar2=None, op0=ALU.mult)
            p.op("vector", "scalar_tensor_tensor", out=c(52, 56), in0=c(48, 52), scalar=c(38, 39), in1=c(52, 56),
                 op0=ALU.mult, op1=ALU.add)
            for g in range(4):
                p.op("vector", "tensor_scalar", out=dw[:, tt, 4 * g:4 * g + 4], in0=c(52, 56), scalar1=c(24 + g, 25 + g),
                     scalar2=None, op0=ALU.mult)
        for e in range(16):
            wein, weout = weins[e % 2], weouts[e % 2]
            for k in range(0, 8, 2):
                p.op("sync", "dma_start", out=S(wein[:, k:k + 2, :], wein.name + "%d" % k), in_=weinv[e, :, k:k + 2, :])
            p.op("sync", "dma_start", out=weout[:], in_=weoutv[e, :, :, :])
            for j in range(4):
                pg_, pu_ = psGU[(j % 2) * 2], psGU[(j % 2) * 2 + 1]
                for (pt, off) in ((pg_, 0), (pu_, 512)):
                    for k in range(8):
                        p.op("tensor", "matmul", out=pt[:, 0:T], lhsT=S(wein[:, k, off + j * 128:off + (j + 1) * 128], wein.name + "%d" % (k // 2 * 2)),
                             rhs=h2T[:, 0:len(tiles), k, :], start=(k == 0), stop=(k == 7), _r=[hk(q) for q in range(len(tiles))])
                sb_ = sgb[j % 2]
                p.op("scalar", "activation", out=sb_[:, 0:T], in_=pg_[:, 0:T], func=AF.Silu)
                p.op("vector", "tensor_tensor", out=S(hidT[:, j, 0:T], hidT.name + "%d" % j), in0=sb_[:, 0:T], in1=pu_[:, 0:T], op=ALU.mult)
            for tt, t in enumerate(tiles):
                for h in range(2):
                    po = psO[h]
                    for j in range(4):
                        p.op("tensor", "matmul", out=po[:, :], lhsT=S(hidT[:, j, tt * 128:(tt + 1) * 128], hidT.name + "%d" % j),
                             rhs=weout[:, j, h * 512:(h + 1) * 512], start=(j == 0), stop=(j == 3))
                    a = S(acc[:, tt, h * 512:(h + 1) * 512], acc.name + "%d_%d" % (tt, h))
                    if e == 0:
                        p.op("vector", "tensor_scalar", out=a, in0=po[:, :], scalar1=dw[:, tt, e:e + 1], scalar2=None, op0=ALU.mult)
                    else:
                        p.op("vector", "scalar_tensor_tensor", out=a, in0=po[:, :], scalar=dw[:, tt, e:e + 1], in1=a,
                             op0=ALU.mult, op1=ALU.add)
        for tt, t in enumerate(tiles):
            r = 1 if t < 2 else 0
            xk = xg.name + "%d" % tt
            for h in range(2):
                a = S(acc[:, tt, h * 512:(h + 1) * 512], acc.name + "%d_%d" % (tt, h))
                xh = S(xg[:, tt, h * 512:(h + 1) * 512], xk)
                p.op("vector", "tensor_tensor", out=a, in0=a, in1=mods[r][:, 2 * D + h * 512:2 * D + (h + 1) * 512], op=ALU.mult)
                p.op("vector", "tensor_tensor", out=xh, in0=xh, in1=a, op=ALU.add)
            X = S(xg[:, tt, :], xk)
            if final:
                p.op("scalar", "activation", out=h2b[:], in_=X, func=AF.Square, accum_out=ss[:])
                p.op("vector", "tensor_scalar", out=ss[:], in0=ss[:], scalar1=1.0 / D, scalar2=RMS_EPS, op0=ALU.mult, op1=ALU.add)
                p.op("scalar", "activation", out=ss[:], in_=ss[:], func=AF.Sqrt)
                p.op("vector", "reciprocal", out=ss[:], in_=ss[:])
                p.op("vector", "scalar_tensor_tensor", out=X, in0=X, scalar=ss[:, 0:1], in1=gf[:], op0=ALU.mult, op1=ALU.mult)
                p.op("sync", "dma_start", out=R["out"][(t - 2) * 128:(t - 1) * 128, :], in_=X)
            else:
                p.op("sync", "dma_start", out=R["xcur"][t * 128:(t + 1) * 128, :], in_=X)
    p.end_phase()


IN_SHAPES = dict(
    xin=[NTT * 128, D], cT=[128, 2, 8], wmod=[DEPTH, D, 6 * D], bmodb=[DEPTH, 128, 6 * D], gn1b=[DEPTH, 128, D],
    gn2b=[DEPTH, 128, D], w_in=[DEPTH, D, PROJ_W], convw=[DEPTH, 4, 128, 4, 5], convb=[DEPTH, 4, 128, 4],
    dtb=[DEPTH, 4, 128, 8], alog=[DEPTH, 4, 128, 8], dskip=[DEPTH, 4, 128, 4], poolw=[DEPTH, 4, 128, 128],
    pscale=[DEPTH, 4, 128, 128], cst=[8, 128, 128], pm2=[4, 9, 128, 128], pm1=[4, 3, 128, 128], rc=[4, 128, NCH],
    ssdg=[DEPTH, 128, D], lng=[DEPTH, 128, 512], lnb=[DEPTH, 128, 512], wsT=[DEPTH, 4, 128, 128], bsT=[DEPTH, 128, 4],
    wbs=[DEPTH, D, D], wbp=[DEPTH, 512, D], wbg=[DEPTH, 512, D], wo=[DEPTH, D, D], wr=[DEPTH, D, 20], brb=[DEPTH, 128, 20],
    wein=[DEPTH, 16, D, D], weout=[DEPTH, 16, 512, D], gfin=[128, D], ident=[128, 128])


def phase_wcast(p, R):
    p.begin_phase("wcast")
    W = R["W"]
    st = [p.psb("st%d" % j, [128, 4, D]) for j in range(3)]
    cv = [p.psb("cv%d" % j, [128, 4, D], BF16) for j in range(3)]
    srcs = []
    for e in range(16):
        vi = W["wein"][e].rearrange("(k q) n -> q k n", q=128)
        vo16 = R["wein16"][e].rearrange("(k q) n -> q k n", q=128)
        srcs += [(vi[:, 0:4, :], vo16[:, 0:4, :]), (vi[:, 4:8, :], vo16[:, 4:8, :])]
        srcs.append((W["weout"][e].rearrange("(k q) n -> q k n", q=128), R["weout16"][e].rearrange("(k q) n -> q k n", q=128)))
    for n, (src, dst) in enumerate(srcs):
        j = n % 3
        p.op("sync", "dma_start", out=st[j][:], in_=src)
        if j == 0:
            p.op("scalar", "activation", out=cv[j][:], in_=st[j][:], func=AF.Copy)
        elif j == 1:
            p.op("vector", "tensor_copy", out=cv[j][:], in_=st[j][:])
        else:
            p.op("gpsimd", "tensor_copy", out=cv[j][:], in_=st[j][:])
        p.op("sync", "dma_start", out=dst, in_=cv[j][:])
    p.end_phase()


LAYERED = ("wmod", "bmodb", "gn1b", "gn2b", "w_in", "convw", "convb", "dtb", "alog", "dskip", "poolw", "pscale", "ssdg", "lng",
           "lnb", "wsT", "bsT", "wbs", "wbp", "wbg", "wo", "wr", "brb", "wein", "weout")


def phase_final(p, R):
    p.begin_phase("fin")
    gf = p.psb("gf", [128, D])
    p.op("sync", "dma_start", out=gf[:], in_=R["W"]["gfin"])
    xt = [p.psb("xt%d" % j, [128, D]) for j in range(3)]
    jk = [p.psb("jk%d" % j, [128, D]) for j in range(2)]
    ss = [p.psb("ss%d" % j, [128, 1]) for j in range(3)]
    for t in range(2, NTT):
        b = t % 3
        p.op("sync", "dma_start", out=xt[b][:], in_=R["xcur"][t * 128:(t + 1) * 128, :])
        emit_rstd(p, xt[b][:], ss[b][:], jk[t % 2][:], RMS_EPS, D)
        p.op("vector", "scalar_tensor_tensor", out=xt[b][:], in0=xt[b][:], scalar=ss[b][:, 0:1], in1=gf[:], op0=ALU.mult, op1=ALU.mult)
        p.op("gpsimd", "dma_start", out=R["out"][(t - 2) * 128:(t - 1) * 128, :], in_=xt[b][:])
    p.end_phase()


BLOB_COLS = 8192


def blob_layout():
    offs, off = {}, 0
    for k in LAYERED:
        sh = IN_SHAPES[k][1:]
        n = int(np.prod(sh))
        offs[k] = (off, n, sh)
        off += (n + 127) // 128 * 128
    rows = (off + BLOB_COLS - 1) // BLOB_COLS
    return offs, rows


def build_fused(depth=DEPTH, phases=('l1', 'l2', 'l3a', 'l3b')):
    nc = bass.Bass("TRN2", target_bir_lowering=False)
    offs, rows = blob_layout()
    W = {k: dram_in(nc, k, sh) for k, sh in IN_SHAPES.items() if k not in LAYERED}
    blob = dram_in(nc, "blob", [DEPTH, rows, BLOB_COLS])
    out = dram_out(nc, "out", [SEQ, D])
    scr = lambda n, sh: nc.dram_tensor(n, list(sh), F32, kind="Internal").ap()
    wcur = scr("wcur", [rows, BLOB_COLS])
    flat = wcur.rearrange("r c -> (r c)")
    for k in LAYERED:
        off, n, sh = offs[k]
        names = "abcde"[:len(sh)]
        W[k] = flat[off:off + n].rearrange("(%s) -> %s" % (" ".join(names), " ".join(names)),
                                           **{nm: int(v) for nm, v in zip(names, sh)})
    R = dict(W=W, cT=W["cT"], out=out,
             xcur=scr("xcur", [NTT * 128, D]), xmid=scr("xmid", [NTT * 128, D]),
             pz=scr("pz", [NTT * 128, D]), pm=scr("pm", [NTT * 128, 1568]), pg=scr("pg", [NTT * 128, 3 * D]),
             xbc_c=scr("xbc_c", [2048, CTX + 4]), xbc_l=scr("xbc_l", [2048, SEQ + 4]),
             yf=scr("yf", [NTT * 128, D]), yb=scr("yb", [NTT * 128, D]), ypool=scr("ypool", [NTT * 128, 512]),
             wein16=nc.dram_tensor("wein16", [16, D, D], BF16, kind="Internal").ap(),
             weout16=nc.dram_tensor("weout16", [16, 512, D], BF16, kind="Internal").ap())
    p = Prog(nc)
    R["pst"] = [p.ps("ps%d" % j) for j in range(8)]
    R["ident"] = p.sb("identsb", [128, 128])
    R["cst"] = p.sb("csts", [128, 8, 128])
    p.begin_phase("init")
    p.op("sync", "dma_start", out=R["ident"][:], in_=W["ident"])
    p.op("sync", "dma_start", out=R["cst"][:], in_=W["cst"].rearrange("c q n -> q c n"))
    zt = p.psb("zeros", [128, 16, 2])
    p.op("vector", "memset", ap=zt[:], constant=0.0)
    for dst, n in ((R["xbc_c"], CTX), (R["xbc_l"], SEQ)):
        v = dst.rearrange("(j q) t -> q j t", q=128)
        p.op("sync", "dma_start", out=v[:, :, 0:2], in_=zt[:])
        p.op("sync", "dma_start", out=v[:, :, n + 2:n + 4], in_=zt[:])
    for j in range(10):
        p.op("sync", "dma_start", out=R["xcur"][j * 1664:(j + 1) * 1664, :], in_=W["xin"][j * 1664:(j + 1) * 1664, :])
    p.end_phase()
    init = p.end_segment()
    p.begin_phase("wload")
    lb = LAP(blob)
    NCP = 8
    step = (rows + NCP - 1) // NCP
    for j in range(NCP):
        r0, r1 = j * step, min(rows, (j + 1) * step)
        p.op("sync", "dma_start", out=wcur[r0:r1, :], in_=lb[r0:r1, :])
    p.end_phase()
    if 'l1' in phases:
        phase_l1(p, R)
    if 'l2' in phases:
        for g in range(4):
            phase_l2(p, R, g)
    if 'l3a' in phases:
        phase_l3a(p, R)
    if 'l3b' in phases or 'wcast' in phases:
        phase_wcast(p, R)
    if 'l3b' in phases:
        phase_l3b(p, R)
    body = p.end_segment()
    phase_final(p, R)
    epi = p.end_segment()
    p.emit_program(init, body, epi, depth)
    return nc


def rep(v, n=128):
    v = np.asarray(v, np.float32).reshape(-1)
    return np.ascontiguousarray(np.broadcast_to(v[None, :], (n, v.shape[0])))


def scan_consts():
    k = np.arange(128)[:, None]
    i = np.arange(128)[None, :]
    c = np.zeros((8, 128, 128), np.float32)
    c[0] = np.eye(128)
    c[1] = 1.0
    c[2] = (k <= i)
    c[3] = (k >= i)
    c[4] = (k > i)
    c[5] = (k < i)
    c[6] = (i >= k)
    c[7] = (i <= k)
    return c


def pool_consts(w):
    lo, hi = w // 2, w - w // 2
    pm2 = np.zeros((9, 128, 128), np.float32)
    k = np.arange(128)
    i = np.arange(128)
    for dl in range(-4, 5):
        rk = 2 * dl + k // 64
        ck = k % 64
        ri = i // 64
        ci = i % 64
        pm2[dl + 4] = ((rk[:, None] >= ri[None, :] - lo) & (rk[:, None] < ri[None, :] + hi) &
                       (ck[:, None] >= ci[None, :] - lo) & (ck[:, None] < ci[None, :] + hi))
    pm1 = np.zeros((3, 128, 128), np.float32)
    for dl in range(-1, 2):
        pk = dl * 128 + k
        pm1[dl + 1] = (pk[:, None] >= i[None, :] - lo) & (pk[:, None] < i[None, :] + hi)

    def cnt(n):
        pos = np.arange(n)
        return (np.clip(pos + hi, 0, n) - np.clip(pos - lo, 0, n)).astype(np.float32)
    c64, c256 = cnt(64), cnt(256)
    rows = np.arange(SEQ) // 64
    cols = np.arange(SEQ) % 64
    rc = np.concatenate([1.0 / cnt(CTX), 1.0 / (c256[rows] * c64[cols])]).astype(np.float32)
    return pm2, pm1, np.ascontiguousarray(rc.reshape(NCH, 128).T)


def host_inputs(W):
    cc = np.ascontiguousarray
    L = DEPTH
    m = {}
    m["wmod"] = W["w_mod"]
    m["bmodb"] = cc(np.stack([rep(W["b_mod"][i]) for i in range(L)]))
    m["gn1b"] = cc(np.stack([rep(W["g_norm1"][i]) for i in range(L)]))
    m["gn2b"] = cc(np.stack([rep(W["g_norm2"][i]) for i in range(L)]))
    m["w_in"] = W["w_in"]
    convw = np.zeros((L, 4, 128, 4, 5), np.float32)
    convb = np.zeros((L, 4, 128, 4), np.float32)
    dtb = np.zeros((L, 4, 128, 8), np.float32)
    alog = np.zeros((L, 4, 128, 8), np.float32)
    dskip = np.zeros((L, 4, 128, 4), np.float32)
    pscale = np.zeros((L, 4, 128, 128), np.float32)
    for i in range(L):
        for g in range(4):
            ch = np.r_[g * 256:(g + 1) * 256, 1024 + g * 128:1024 + (g + 1) * 128, 1536 + g * 128:1536 + (g + 1) * 128]
            convw[i, g] = W["conv_w"][i][:, ch].T.reshape(4, 128, 5).transpose(1, 0, 2)
            convb[i, g] = W["conv_b"][i][ch].reshape(4, 128).T
            hs = slice(4 * g, 4 * g + 4)
            dtb[i, g] = rep(np.concatenate([W["dt_bias"][i][0, hs], W["dt_bias"][i][1, hs]]))
            alog[i, g] = rep(np.concatenate([W["a_log"][i][0, hs], W["a_log"][i][1, hs]]))
            dskip[i, g] = rep(W["d_skip"][i][hs])
            pscale[i, g] = rep(W["pool_scale"][i][g * 128:(g + 1) * 128])
    m.update(convw=convw, convb=convb, dtb=dtb, alog=alog, dskip=dskip, pscale=pscale, poolw=W["pool_w"])
    pcs = [pool_consts(w) for w in POOL_WINDOWS]
    m["cst"] = scan_consts()
    m["pm2"] = cc(np.stack([c[0] for c in pcs]))
    m["pm1"] = cc(np.stack([c[1] for c in pcs]))
    m["rc"] = cc(np.stack([c[2] for c in pcs]))
    m["ssdg"] = cc(np.stack([rep(W["ssd_norm_g"][i]) for i in range(L)]))
    m["lng"] = cc(np.stack([rep(W["gmlp_ln_g"][i]) for i in range(L)]))
    m["lnb"] = cc(np.stack([rep(W["gmlp_ln_b"][i]) for i in range(L)]))
    m["wsT"] = cc(W["gmlp_ws"].transpose(0, 1, 3, 2))
    m["bsT"] = cc(W["gmlp_bs"].transpose(0, 2, 1))
    m.update(wbs=W["w_br_ssd"], wbp=W["w_br_pool"], wbg=W["w_br_gmlp"], wo=W["w_o"])
    m["wr"] = cc(np.concatenate([W["w_rg"], W["w_re"]], 2))
    m["brb"] = cc(np.stack([rep(np.concatenate([W["b_rg"][i], W["b_re"][i]])) for i in range(L)]))
    m.update(wein=W["w_e_in"], weout=W["w_e_out"], gfin=rep(W["g_final"]), ident=np.eye(128, dtype=np.float32))
    offs, rows = blob_layout()
    blob = np.zeros((L, rows * BLOB_COLS), np.float32)
    for k in LAYERED:
        off, n, sh = offs[k]
        a = np.asarray(m.pop(k), np.float32)
        assert list(a.shape[1:]) == list(sh), (k, a.shape, sh)
        blob[:, off:off + n] = a.reshape(L, n)
    m["blob"] = blob.reshape(L, rows, BLOB_COLS)
    return m


_NC = {}


def kernel(**inp):
    W = {k: np.asarray(v, np.float32) for k, v in inp.items()}
    shared = host_inputs(W)
    maps = []
    for k in range(NCORE):
        b = k // max(1, NCORE // 2)
        cv = np.stack([W["c"][b], W["c_ctx"]], 0)
        m = dict(shared)
        m["xin"] = np.ascontiguousarray(np.concatenate([W["ctx"][b], W["x"][b]], 0))
        m["cT"] = np.ascontiguousarray(cv.reshape(2, 8, 128).transpose(2, 0, 1))
        maps.append(m)
    if "nc" not in _NC:
        _NC["nc"] = build_fused()
    res = run_bass_kernel_spmd(_NC["nc"], maps, core_ids=list(range(NCORE)))
    return np.stack([res.results[0]["out"], res.results[NCORE // 2]["out"]], 0)
```
